# Optimizing a Trainium2 kernel written in Bass

```python
import math
import jax, jax.numpy as jnp
from jax import lax
import numpy as np

D_MODEL = 2048
BATCH = 2
SEQ = 8192
DEPTH = 1

HEAD_DIM = 128
N_HEADS_SB = 8
N_HEADS_DSA = 8
N_IDX_HEADS = 16
IDX_DIM = 64
TOPK_MAX = 256
D_FF = 5632
CONV_WIDTH = 3
N_BUCKETS = 32
MAX_DISTANCE = 128
Q_BLOCK = 128
EPS = 1e-6
W_SB = N_HEADS_SB * HEAD_DIM
W_DSA = N_HEADS_DSA * HEAD_DIM
IN_SIZES = (W_SB, W_SB, W_SB, W_DSA, W_DSA, W_DSA,
            N_IDX_HEADS * IDX_DIM, IDX_DIM, N_IDX_HEADS, D_MODEL, D_MODEL)
P_IN = 3 * W_SB + 3 * W_DSA + N_IDX_HEADS * IDX_DIM + IDX_DIM + N_IDX_HEADS + 2 * D_MODEL

kernel_name = 'hybrid_stickbreak_dsa_convffn_block'


def rms_norm(x, g):
    xf = x.astype(jnp.float32)
    y = xf * lax.rsqrt(jnp.mean(xf * xf, axis=-1, keepdims=True) + EPS)
    return (y * g.astype(jnp.float32)).astype(x.dtype)


def modulate(h, shift, scale):
    return h * (1.0 + scale[:, None, :]) + shift[:, None, :]


def split_columns(z):
    cuts, acc = [], 0
    for s in IN_SIZES[:-1]:
        acc += s
        cuts.append(acc)
    return jnp.split(z, cuts, axis=-1)


def rel_bucket(dist):
    n = jnp.maximum(dist, 0)
    max_exact = N_BUCKETS // 2
    nf = jnp.maximum(n, 1).astype(jnp.float32)
    large = max_exact + (jnp.log(nf / max_exact) / math.log(MAX_DISTANCE / max_exact)
                         * (N_BUCKETS - max_exact)).astype(jnp.int32)
    large = jnp.minimum(large, N_BUCKETS - 1)
    return jnp.where(n < max_exact, n, large)


def stick_breaking_attention(q, k, v):
    B, S, H, Dh = q.shape
    n_blk = S // Q_BLOCK
    kf = k.astype(jnp.float32)
    vf = v.astype(jnp.float32)
    key_pos = jnp.arange(S)
    scale = HEAD_DIM ** -0.5

    def block(i):
        start = i * Q_BLOCK
        qb = lax.dynamic_slice_in_dim(q, start, Q_BLOCK, axis=1).astype(jnp.float32)
        z = jnp.einsum('bqhd,bshd->bhqs', qb, kf) * scale
        q_pos = start + jnp.arange(Q_BLOCK)
        causal = key_pos[None, :] < q_pos[:, None]
        log_beta = jax.nn.log_sigmoid(z)
        log_keep = jnp.where(causal, jax.nn.log_sigmoid(-z), 0.0)
        suffix = lax.cumsum(log_keep, axis=3, reverse=True) - log_keep
        a = jnp.where(causal, jnp.exp(log_beta + suffix), 0.0)
        return jnp.einsum('bhqs,bshd->bqhd', a, vf)

    out = lax.map(block, jnp.arange(n_blk))
    return out.transpose(1, 0, 2, 3, 4).reshape(B, S, H * Dh).astype(q.dtype)


def indexer_sparse_attention(q, k, v, q_idx, k_idx, w_idx, rel_bias):
    B, S, H, Dh = q.shape
    n_blk = S // Q_BLOCK
    n_sel = min(TOPK_MAX, S // 4)
    kf = k.astype(jnp.float32)
    vf = v.astype(jnp.float32)
    kif = k_idx.astype(jnp.float32)
    key_pos = jnp.arange(S)
    scale = HEAD_DIM ** -0.5
    idx_scale = IDX_DIM ** -0.5
    gather = jax.vmap(lambda arr, ix: arr[ix])

    def block(i):
        start = i * Q_BLOCK
        q_pos = start + jnp.arange(Q_BLOCK)
        qi = lax.dynamic_slice_in_dim(q_idx, start, Q_BLOCK, axis=1).astype(jnp.float32)
        wi = lax.dynamic_slice_in_dim(w_idx, start, Q_BLOCK, axis=1).astype(jnp.float32)
        per_head = jax.nn.relu(jnp.einsum('bqhd,bsd->bqhs', qi, kif) * idx_scale)
        score = jnp.einsum('bqh,bqhs->bqs', wi, per_head)
        causal = key_pos[None, :] <= q_pos[:, None]
        score = jnp.where(causal[None], score, -jnp.inf)
        _, sel = lax.top_k(score, n_sel)
        valid = sel <= q_pos[None, :, None]
        k_sel = gather(kf, sel)
        v_sel = gather(vf, sel)
        qb = lax.dynamic_slice_in_dim(q, start, Q_BLOCK, axis=1).astype(jnp.float32)
        logits = jnp.einsum('bqhd,bqkhd->bhqk', qb, k_sel) * scale
        bias = rel_bias.astype(jnp.float32)[rel_bucket(q_pos[None, :, None] - sel)]
        logits = logits + bias.transpose(0, 3, 1, 2)
        logits = jnp.where(valid[:, None], logits, -jnp.inf)
        p = jax.nn.softmax(logits, axis=-1)
        return jnp.einsum('bhqk,bqkhd->bqhd', p, v_sel)

    out = lax.map(block, jnp.arange(n_blk))
    return out.transpose(1, 0, 2, 3, 4).reshape(B, S, H * Dh).astype(q.dtype)


def causal_dwconv(a, w, b):
    S = a.shape[1]
    ap = jnp.pad(a, ((0, 0), (CONV_WIDTH - 1, 0), (0, 0)))
    y = b
    for j in range(CONV_WIDTH):
        y = y + ap[:, j:j + S, :] * w[j]
    return y


def setup_inputs(seed: int = 0) -> dict:
    key = jax.random.key(seed)
    ks = jax.random.split(key, 20)
    f32 = jnp.float32

    def nrm(k, shape, fan_in):
        return jax.random.normal(k, shape, f32) * fan_in ** -0.5

    return {
        'x': jax.random.normal(ks[0], (BATCH, SEQ, D_MODEL), f32),
        'c': jax.random.normal(ks[1], (BATCH, D_MODEL), f32),
        'w_ada': 0.5 * nrm(ks[2], (DEPTH, D_MODEL, 6 * D_MODEL), D_MODEL),
        'b_ada': 0.01 * jax.random.normal(ks[3], (DEPTH, 6 * D_MODEL), f32),
        'g_mix': 1.0 + 0.05 * jax.random.normal(ks[4], (DEPTH, D_MODEL), f32),
        'w_in': nrm(ks[5], (DEPTH, D_MODEL, P_IN), D_MODEL),
        'w_o_sb': nrm(ks[6], (DEPTH, W_SB, D_MODEL), W_SB),
        'w_o_dsa': nrm(ks[7], (DEPTH, W_DSA, D_MODEL), W_DSA),
        'w_out': nrm(ks[8], (DEPTH, D_MODEL, D_MODEL), D_MODEL),
        'rel_bias': 0.5 * jax.random.normal(ks[9], (N_BUCKETS, N_HEADS_DSA), f32),
        'g_ffn': 1.0 + 0.05 * jax.random.normal(ks[10], (DEPTH, D_MODEL), f32),
        'w_gate': nrm(ks[11], (DEPTH, D_MODEL, D_FF), D_MODEL),
        'w_up': nrm(ks[12], (DEPTH, D_MODEL, D_FF), D_MODEL),
        'conv_w': nrm(ks[13], (DEPTH, CONV_WIDTH, D_FF), CONV_WIDTH),
        'conv_b': 0.01 * jax.random.normal(ks[14], (DEPTH, D_FF), f32),
        'w_down': nrm(ks[15], (DEPTH, D_FF, D_MODEL), D_FF),
        'g_final': 1.0 + 0.05 * jax.random.normal(ks[16], (D_MODEL,), f32),
    }


def reference(x, c, w_ada, b_ada, g_mix, w_in, w_o_sb, w_o_dsa, w_out, rel_bias,
              g_ffn, w_gate, w_up, conv_w, conv_b, w_down, g_final):
    B, S, _ = x.shape
    c_act = jax.nn.silu(c)
    for l in range(DEPTH):
        mod = c_act @ w_ada[l] + b_ada[l]
        sh1, sc1, gt1, sh2, sc2, gt2 = jnp.split(mod, 6, axis=-1)

        h = modulate(rms_norm(x, g_mix[l]), sh1, sc1)
        z = h @ w_in[l]
        (q_sb, k_sb, v_sb, q_ds, k_ds, v_ds, q_ix, k_ix, w_ix,
         gate_sb, gate_ds) = split_columns(z)
        hs = (B, S, N_HEADS_SB, HEAD_DIM)
        hd = (B, S, N_HEADS_DSA, HEAD_DIM)
        o_sb = stick_breaking_attention(q_sb.reshape(hs), k_sb.reshape(hs), v_sb.reshape(hs))
        o_ds = indexer_sparse_attention(
            q_ds.reshape(hd), k_ds.reshape(hd), v_ds.reshape(hd),
            q_ix.reshape(B, S, N_IDX_HEADS, IDX_DIM), k_ix,
            w_ix * (N_IDX_HEADS ** -0.5), rel_bias)
        merged = (jax.nn.sigmoid(gate_sb) * (o_sb @ w_o_sb[l])
                  + jax.nn.sigmoid(gate_ds) * (o_ds @ w_o_dsa[l]))
        x = x + gt1[:, None, :] * (merged @ w_out[l])

        h = modulate(rms_norm(x, g_ffn[l]), sh2, sc2)
        a = causal_dwconv(h @ w_gate[l], conv_w[l], conv_b[l])
        y = (jax.nn.silu(a) * (h @ w_up[l])) @ w_down[l]
        x = x + gt2[:, None, :] * y
    return rms_norm(x, g_final)
```

```python
import math
import contextlib
import numpy as np
import concourse.bass as bass
import concourse.mybir as mybir
from concourse.bass_utils import run_bass_kernel_spmd

F32 = mybir.dt.float32
BF16 = mybir.dt.bfloat16
AF = mybir.ActivationFunctionType
ALU = mybir.AluOpType

D = 2048
SEQ = 8192
FC = 16
DFF = 5632
NFF = 44
PIN = 11344
NTILE = 4
TW = 512
WCW = 2562
LGV = 2690
NKB = 64
SCALE = 128.0 ** -0.5
WIS_SCALE = (16.0 ** -0.5) * (64.0 ** -0.5)
NEG = -3.0e38
BIG = 1.0e30

C_QSB, C_KSB, C_VSB, C_QDS, C_KDS, C_VDS, C_QIX, C_KIX, C_WIX, C_GSB, C_GDS = (
    0, 1024, 2048, 3072, 4096, 5120, 6144, 7168, 7232, 7248, 9296)


class DSem:
    def __init__(self, h):
        self.h = h
        self.count = 0


class Tk:
    def __init__(self, t, name):
        self.t = t
        self.name = name
        self.wev = {}
        self.readers = {}

    def __getitem__(self, idx):
        return self.t[idx]


NRING = 40


class Sched:
    def __init__(self, nc):
        self.nc = nc
        self.eng = {"pe": nc.tensor, "act": nc.scalar, "dve": nc.vector, "pool": nc.gpsimd, "sp": nc.sync}
        self.sem = {k: nc.alloc_semaphore(name="sem_" + k) for k in ("pe", "act", "dve", "pool")}
        self.cnt = {k: 0 for k in self.sem}
        self.seen = {k: {} for k in self.eng}
        self.ring = [DSem(nc.alloc_semaphore(name="dsem%d" % i)) for i in range(NRING)]
        self.all_dsems = self.ring
        self.ri = 0
        self.n_inst = 0
        self.n_wait = 0

    def release(self, tiles):
        pass

    def _wait(self, eng, deps):
        e = self.eng[eng]
        for key, val in deps:
            if key == "pe" and eng == "pe":
                continue
            if self.seen[eng].get(key, 0) >= val:
                continue
            sem = self.sem[key] if isinstance(key, str) else key.h
            e.wait_ge(sem, val)
            self.n_wait += 1
            self.seen[eng][key] = val

    @staticmethod
    def _deps(reads, writes, partial=False):
        deps = []
        for t in reads:
            deps.extend(t.wev.items())
        for t in writes:
            for k, v in t.wev.items():
                if partial and not isinstance(k, str):
                    continue
                deps.append((k, v))
            deps.extend(t.readers.items())
        return deps

    def op(self, eng, fn, reads=(), writes=()):
        self._wait(eng, self._deps(reads, writes))
        inst = fn(self.eng[eng])
        self.cnt[eng] += 1
        self.n_inst += 1
        inst.then_inc(self.sem[eng], 1)
        val = self.cnt[eng]
        for t in reads:
            t.readers[eng] = val
        for t in writes:
            t.wev = {eng: val}
            t.readers = {}

    def dma(self, q, out_ap, in_ap, reads=(), writes=(), partial=False):
        dst = writes[0]
        ds = self.ring[self.ri % NRING]
        self.ri += 1
        deps = self._deps(reads, writes, partial)
        if ds.count > 0:
            deps.append((ds, ds.count))
        self._wait(q, deps)
        inst = self.eng[q].dma_start(out=out_ap, in_=in_ap)
        inst.then_inc(ds.h, 16)
        ds.count += 16
        self.n_inst += 1
        for t in reads:
            t.readers[ds] = ds.count
        if partial:
            dst.wev[ds] = ds.count
        else:
            dst.wev = {ds: ds.count}
        dst.readers = {}

    def barrier(self, engines=None):
        deps = [(k, self.cnt[k]) for k in self.sem if self.cnt[k] > 0]
        deps += [(ds, ds.count) for ds in self.all_dsems if ds.count > 0]
        for eng in (engines or self.eng):
            self._wait(eng, deps)


class Scope:
    def __init__(self, K, name):
        self.K = K
        self.name = name
        self.es = contextlib.ExitStack()
        self.tiles = []

    def sb(self, name, shape, dt):
        t = Tk(self.es.enter_context(self.K.nc.sbuf_tensor(self.name + "_" + name, list(shape), dt)), name)
        self.tiles.append(t)
        return t

    def ps(self, name, shape, dt=F32):
        t = Tk(self.es.enter_context(self.K.nc.psum_tensor(self.name + "_" + name, list(shape), dt)), name)
        self.tiles.append(t)
        return t

    def __enter__(self):
        return self

    def __exit__(self, *a):
        if a[0] is None or a[0] is StopBuild:
            self.K.S.barrier()
        self.es.close()
        return False


def sub_ap(ap, off, dims):
    return bass.AP(ap.tensor, ap.offset + off, [list(ap.ap[0])] + [list(d) for d in dims])


class StopBuild(Exception):
    pass


class Rot:
    def __init__(self, tiles):
        self.tiles = tiles
        self.i = 0

    def next(self):
        t = self.tiles[self.i % len(self.tiles)]
        self.i += 1
        return t


class Builder:
    def __init__(self, stop_after=None, debug=False):
        self.stop_after = stop_after
        self.debug = debug
        self.nc = bass.Bass("TRN2", target_bir_lowering=False)
        self.S = Sched(self.nc)
        self.dr = {}

    def din(self, name, shape):
        t = self.nc.dram_tensor(name, list(shape), F32, kind="ExternalInput")
        self.dr[name] = Tk(t.ap(), name)
        return self.dr[name]

    def dscr(self, name, shape, dt):
        kind = "ExternalOutput" if self.debug else "Internal"
        t = self.nc.dram_tensor(name, list(shape), dt, kind=kind)
        self.dr[name] = Tk(t.ap(), name)
        return self.dr[name]

    def mm(self, ot, o, lt, l, rt, r, start, stop):
        self.S.op("pe", lambda e: e.matmul(o, lhsT=l, rhs=r, start=start, stop=stop), reads=[lt, rt], writes=[ot])

    def tr(self, ot, o, it, i, idt, ident):
        self.S.op("pe", lambda e: e.transpose(o, i, ident), reads=[it, idt], writes=[ot])

    def act(self, ot, o, it, i, func, scale=1.0, bias=None, extra_reads=()):
        kw = {}
        if bias is not None:
            kw["bias"] = bias
        self.S.op("act", lambda e: e.activation(out=o, in_=i, func=func, scale=scale, **kw),
                  reads=[it] + list(extra_reads), writes=[ot])

    def tt(self, eng, ot, o, at, a, bt, b, op):
        self.S.op(eng, lambda e: e.tensor_tensor(out=o, in0=a, in1=b, op=op), reads=[at, bt], writes=[ot])

    def ts(self, eng, ot, o, it, i, s1, op0, s2=None, op1=None, extra_reads=()):
        if op1 is None:
            self.S.op(eng, lambda e: e.tensor_scalar(out=o, in0=i, scalar1=s1, scalar2=None, op0=op0),
                      reads=[it] + list(extra_reads), writes=[ot])
        else:
            self.S.op(eng, lambda e: e.tensor_scalar(out=o, in0=i, scalar1=s1, scalar2=s2, op0=op0, op1=op1),
                      reads=[it] + list(extra_reads), writes=[ot])

    def stt(self, ot, o, at, a, scalar, bt, b, op0, op1, extra_reads=()):
        self.S.op("dve", lambda e: e.scalar_tensor_tensor(out=o, in0=a, scalar=scalar, in1=b, op0=op0, op1=op1),
                  reads=[at, bt] + list(extra_reads), writes=[ot])

    def cp(self, eng, ot, o, it, i):
        if eng == "act":
            self.act(ot, o, it, i, AF.Copy)
        else:
            self.S.op(eng, lambda e: e.tensor_copy(out=o, in_=i), reads=[it], writes=[ot])

    def memset(self, eng, ot, o, val):
        self.S.op(eng, lambda e: e.memset(o, val), reads=[], writes=[ot])

    def build(self):
        nc, S = self.nc, self.S
        din = self.din
        xq = din("xq", [2056, D]); xkv = din("xkv", [SEQ, D]); cT = din("cT", [128, 16])
        w_ada = din("w_ada", [D, 6 * D]); b_adaT = din("b_adaT", [128, 96])
        gmixT = din("g_mixT", [128, 16]); gffnT = din("g_ffnT", [128, 16]); gfinT = din("g_finT", [128, 16])
        w_in = din("w_in", [D, PIN]); w_o_sb = din("w_o_sb", [1024, D]); w_o_dsa = din("w_o_dsa", [1024, D])
        w_out = din("w_out", [D, D]); rel_bias = din("rel_bias", [32, 8])
        w_gate = din("w_gate", [D, DFF]); w_up = din("w_up", [D, DFF])
        convT = din("convT", [128, NFF * 3]); convb = din("convb", [128, NFF]); w_down = din("w_down", [DFF, D])
        identd = din("ident", [128, 128]); trid = din("tri", [128, 128])
        wcd = din("wc", [128, WCW]); wcapd = din("wcap", [128, WCW]); hcapd = din("hcap", [8, SEQ])
        hvd = din("hv", [128, 8]); ohbd = din("ohb", [32, LGV]); czd = din("cz", [8, LGV])
        yout = self.nc.dram_tensor("y", [2048, D], F32, kind="ExternalOutput")
        yout = Tk(yout.ap(), "y"); self.dr["y"] = yout
        KT_sb = self.dscr("KT_sb", [8, 128, SEQ], BF16); V_sb = self.dscr("V_sb", [NKB, 128, 1024], BF16)
        KT_ds = self.dscr("KT_ds", [8, 128, SEQ], BF16); V_ds = self.dscr("V_ds", [NKB, 128, 1024], BF16)
        KI = self.dscr("KI", [64, SEQ], BF16); gv = self.dscr("gv", [8, LGV], BF16)
        mTd = self.dscr("mTd", [NKB, 128, TW], BF16)
        self.P = Scope(self, "P")
        P = self.P
        with P:
            self._build_body(locals())
        return nc

    def _build_body(self, L):
        nc, S, P = self.nc, self.S, self.P
        g = L
        ident = P.sb("ident", [128, 128], F32); identb = P.sb("identb", [128, 128], BF16)
        trib = P.sb("trib", [128, 128], BF16); onesb = P.sb("onesb", [128, 128], BF16)
        onesf = P.sb("onesf", [128, 128], F32)
        wc = P.sb("wc", [128, WCW], BF16)
        hv = P.sb("hv", [128, 8], F32)
        modT = P.sb("modT", [128, 96], F32); gm1 = P.sb("gm1", [128, 16], F32); gm2 = P.sb("gm2", [128, 16], F32)
        gfin = P.sb("gfin", [128, 16], F32); cw = P.sb("cw", [128, NFF * 3], F32); cb = P.sb("cb", [128, NFF], F32)
        epsc = P.sb("epsc", [128, 1], F32)
        FHc = P.sb("FHc", [128, NKB, 8], BF16); FHd = P.sb("FHd", [128, 8, NKB * 8], BF16)
        a_halo = P.sb("a_halo", [128, NFF, 8], F32)
        self.c = dict(ident=ident, identb=identb, trib=trib, onesb=onesb, onesf=onesf, wc=wc,
                      hv=hv, modT=modT, gm1=gm1, gm2=gm2, gfin=gfin, cw=cw, cb=cb, epsc=epsc, FHc=FHc, FHd=FHd,
                      a_halo=a_halo)
        self.g = g
        try:
            self.setup()
            self.check_stop("setup")
            self.kv_phase()
            self.check_stop("kv")
            self.query_tile(None)
            self.check_stop("halo")
            for r in range(NTILE):
                self.query_tile(r)
                self.check_stop("tile%d" % r)
        except StopBuild:
            pass
        self.finish_debug()

    def check_stop(self, name):
        if self.stop_after == name:
            raise StopBuild()

    def finish_debug(self):
        self.S.barrier()

    def setup(self):
        S, c, g = self.S, self.c, self.g
        with Scope(self, "su") as sc:
            st32 = Rot([sc.sb("st32_%d" % i, [128, 2048], F32) for i in range(3)])
            S.dma("sp", c["ident"][:], g["identd"][:, :], reads=[g["identd"]], writes=[c["ident"]])
            S.dma("sp", c["hv"][:], g["hvd"][:, :], reads=[g["hvd"]], writes=[c["hv"]])
            S.dma("sp", c["gfin"][:], g["gfinT"][:, :], reads=[g["gfinT"]], writes=[c["gfin"]])
            S.dma("sp", c["cw"][:], g["convT"][:, :], reads=[g["convT"]], writes=[c["cw"]])
            S.dma("sp", c["cb"][:], g["convb"][:, :], reads=[g["convb"]], writes=[c["cb"]])
            self.memset("dve", c["onesb"], c["onesb"][:], 1.0)
            self.memset("dve", c["onesf"], c["onesf"][:], 1.0)
            self.memset("dve", c["epsc"], c["epsc"][:], 1e-6)
            t = st32.next()
            S.dma("sp", t[:, 0:128], g["identd"][:, :], reads=[g["identd"]], writes=[t])
            self.cp("dve", c["identb"], c["identb"][:], t, t[:, 0:128])
            t = st32.next()
            S.dma("sp", t[:, 0:128], g["trid"][:, :], reads=[g["trid"]], writes=[t])
            self.cp("dve", c["trib"], c["trib"][:], t, t[:, 0:128])
            for c0 in range(0, WCW, 2048):
                n = min(2048, WCW - c0)
                t = st32.next()
                S.dma("sp", t[:, 0:n], g["wcd"][:, c0:c0 + n], reads=[g["wcd"]], writes=[t])
                self.cp("dve", c["wc"], c["wc"][:, c0:c0 + n], t, t[:, 0:n])
            cact = sc.sb("cact", [128, 16], F32); csig = sc.sb("csig", [128, 16], F32)
            badd = sc.sb("badd", [128, 96], F32); gmx = sc.sb("gmx", [128, 16], F32); gff = sc.sb("gff", [128, 16], F32)
            S.dma("sp", cact[:], g["cT"][:, :], reads=[g["cT"]], writes=[cact])
            S.dma("sp", badd[:], g["b_adaT"][:, :], reads=[g["b_adaT"]], writes=[badd])
            S.dma("sp", gmx[:], g["gmixT"][:, :], reads=[g["gmixT"]], writes=[gmx])
            S.dma("sp", gff[:], g["gffnT"][:, :], reads=[g["gffnT"]], writes=[gff])
            self.act(csig, csig[:], cact, cact[:], AF.Sigmoid)
            self.tt("dve", cact, cact[:], cact, cact[:], csig, csig[:], ALU.mult)
            modp = sc.ps("modp", [128, 96], F32)
            for n in range(96):
                t = st32.next()
                S.dma("sp", t[:, :].rearrange("p (k n) -> p k n", k=16),
                      g["w_ada"][:, 128 * n:128 * n + 128].rearrange("(k p) n -> p k n", p=128),
                      reads=[g["w_ada"]], writes=[t])
                for k in range(16):
                    self.mm(modp, modp[:, n:n + 1], t, t[:, 128 * k:128 * k + 128], cact, cact[:, k:k + 1],
                            start=(k == 0), stop=(k == 15))
            self.tt("dve", c["modT"], c["modT"][:], modp, modp[:], badd, badd[:], ALU.add)
            m = c["modT"]
            self.stt(c["gm1"], c["gm1"][:], m, m[:, 16:32], 1.0, gmx, gmx[:], ALU.add, ALU.mult)
            self.stt(c["gm2"], c["gm2"][:], m, m[:, 64:80], 1.0, gff, gff[:], ALU.add, ALU.mult)
            rb = sc.sb("rb", [32, 8], F32); nb31 = sc.sb("nb31", [8, 1], F32)
            ohb = sc.sb("ohb", [32, LGV], F32); cz = sc.sb("cz", [8, LGV], F32)
            gvs = sc.sb("gvs", [8, LGV], F32); gvb = sc.sb("gvb", [8, LGV], BF16)
            S.dma("sp", rb[:], g["rel_bias"][:, :], reads=[g["rel_bias"]], writes=[rb])
            S.dma("sp", nb31[:], g["rel_bias"][31:32, :].rearrange("a h -> h a"), reads=[g["rel_bias"]], writes=[nb31])
            S.dma("sp", ohb[:], g["ohbd"][:, :], reads=[g["ohbd"]], writes=[ohb])
            S.dma("sp", cz[:], g["czd"][:, :], reads=[g["czd"]], writes=[cz])
            self.ts("dve", nb31, nb31[:], nb31, nb31[:], -1.0, ALU.mult)
            gps = sc.ps("gps", [8, 512], F32)
            for c0 in range(0, LGV, 512):
                n = min(512, LGV - c0)
                self.mm(gps, gps[:, 0:n], rb, rb[:], ohb, ohb[:, c0:c0 + n], start=True, stop=True)
                self.act(gvs, gvs[:, c0:c0 + n], gps, gps[:, 0:n], AF.Exp, bias=nb31[:, 0:1], extra_reads=[nb31])
            self.tt("dve", gvb, gvb[:], gvs, gvs[:], cz, cz[:], ALU.mult)
            S.dma("sp", g["gv"][:, :], gvb[:], reads=[gvb], writes=[g["gv"]])
            wd_all = sc.sb("wd_all", [128, 8, WCW], BF16)
            for h in range(8):
                S.dma("sp", wd_all[:, h, :], bass.AP(g["gv"].t.tensor, h * LGV, [[1, 128], [1, WCW]]),
                      reads=[g["gv"]], writes=[wd_all], partial=True)
            FHc, FHd = c["FHc"], c["FHd"]
            for r in range(4):
                lo = max(0, 48 - 16 * r); hi = min(63, 68 - 16 * r)
                nn = hi - lo + 1
                c0 = 128 * (lo + 16 * r - 48)
                targets = [(FHc, lambda a, b: FHc[:, a:b, 2 * r:2 * r + 2], c["wc"], c["wc"][:, 0:WCW])]
                for h in range(8):
                    targets.append((FHd,
                                    (lambda a, b, h=h: FHd[:, h, :].rearrange("p (k e) -> p k e", e=8)[:, a:b, 2 * r:2 * r + 2]),
                                    wd_all, wd_all[:, h, :]))
                for (dst, dfn, srct, srcap) in targets:
                    if lo > 0:
                        self.memset("dve", dst, dfn(0, lo), 0.0)
                    if hi < 63:
                        self.memset("dve", dst, dfn(hi + 1, 64), 1.0)
                    self.cp("dve", dst, dfn(lo, hi + 1), srct, sub_ap(srcap, c0, [[128, nn], [1, 2]]))
            if self.debug:
                dbg = self.dscr("dbg_mod", [128, 96], F32)
                S.dma("sp", dbg[:, :], c["modT"][:], reads=[c["modT"]], writes=[dbg])
                dbg2 = self.dscr("dbg_fhd", [128, 8 * NKB * 8], BF16)
                S.dma("sp", dbg2[:, :], FHd[:].rearrange("p h k -> p (h k)"), reads=[FHd], writes=[dbg2])
                dbg3 = self.dscr("dbg_fhc", [128, NKB * 8], BF16)
                S.dma("sp", dbg3[:, :], FHc[:].rearrange("p k e -> p (k e)"), reads=[FHc], writes=[dbg3])

    def wload(self, src, rows_ap_fn, nk, ncols, dst, dst_ap):
        per = max(1, 2048 // ncols)
        for k0 in range(0, nk, per):
            k1 = min(nk, k0 + per)
            t = self.wst.next()
            n = (k1 - k0) * ncols
            self.S.dma("sp", t[:, 0:n].rearrange("p (k n) -> p k n", n=ncols), rows_ap_fn(k0, k1),
                       reads=[src], writes=[t])
            self.cp(self.cast_eng(), dst, dst_ap(k0, k1), t, t[:, 0:n].rearrange("p (k n) -> p k n", n=ncols))

    def cast_eng(self):
        self._ce = getattr(self, "_ce", 0) + 1
        return "pool" if self._ce % 4 == 0 else "act"

    def wload_cols(self, wt, c0, ncols, dst, nk=16, dcol0=0):
        self.wload(wt, lambda k0, k1: wt[128 * k0:128 * k1, c0:c0 + ncols].rearrange("(k p) n -> p k n", p=128),
                   nk, ncols, dst, lambda k0, k1: dst[:, k0:k1, dcol0:dcol0 + ncols])

    def norm_tile(self, sc, xsrc, row0, subs, xT, hT, hcol0, gm, sh_col0, do_norm=True):
        S, c = self.S, self.c
        Nt = sum(n for _, n in subs)
        col = 0
        for (ro, nr) in subs:
            xr = self.xrow.next()
            S.dma("sp", xr[0:nr, :], xsrc[row0 + ro:row0 + ro + nr, :], reads=[xsrc], writes=[xr])
            for gq in range(4):
                pt = self.ptr.next()
                for i in range(4):
                    fc = 4 * gq + i
                    self.tr(pt, pt[:, i * nr:(i + 1) * nr], xr, xr[0:nr, 128 * fc:128 * fc + 128],
                            c["ident"], c["ident"][0:nr, 0:nr])
                self.cp("act", xT, xT[:, 4 * gq:4 * gq + 4, col:col + nr],
                        pt, pt[:, 0:4 * nr].rearrange("p (i n) -> p i n", n=nr))
            col += nr
        if do_norm:
            self.norm_from_xT(sc, xT, Nt, hT, hcol0, gm, sh_col0)

    def norm_stats(self, xT, Nt):
        c = self.c
        ss = self.pss.next()
        for fc in range(FC):
            sq = self.sqb.next()
            self.act(sq, sq[:, 0:Nt], xT, xT[:, fc, 0:Nt], AF.Square)
            self.mm(ss, ss[:, 0:Nt], c["onesf"], c["onesf"][:], sq, sq[:, 0:Nt], start=(fc == 0), stop=(fc == FC - 1))
        rs = self.rstd
        self.act(rs, rs[:, 0:Nt], ss, ss[:, 0:Nt], AF.Sqrt, scale=1.0 / D, bias=c["epsc"][:, 0:1],
                 extra_reads=[c["epsc"]])
        self.S.op("dve", lambda e: e.reciprocal(out=rs[:, 0:Nt], in_=rs[:, 0:Nt]), reads=[rs], writes=[rs])
        return rs

    def norm_from_xT(self, sc, xT, Nt, hT, hcol0, gm, sh_col0):
        c = self.c
        rs = self.norm_stats(xT, Nt)
        for fc in range(FC):
            tmp = self.sqb.next()
            self.tt("dve", tmp, tmp[:, 0:Nt], xT, xT[:, fc, 0:Nt], rs, rs[:, 0:Nt], ALU.mult)
            self.act(hT, hT[:, fc, hcol0:hcol0 + Nt], tmp, tmp[:, 0:Nt], AF.Identity, scale=gm[:, fc:fc + 1],
                     bias=c["modT"][:, sh_col0 + fc:sh_col0 + fc + 1], extra_reads=[gm, c["modT"]])

    def kv_phase(self):
        S, c, g = self.S, self.c, self.g
        NSUP = 1024
        with Scope(self, "kv") as sc:
            self.wst = Rot([sc.sb("wst%d" % i, [128, 2048], F32) for i in range(3)])
            self.xrow = Rot([sc.sb("xrow%d" % i, [128, D], F32) for i in range(2)])
            self.ptr = Rot([sc.ps("ptr%d" % i, [128, 512], F32) for i in range(2)])
            self.pss = Rot([sc.ps("pss", [128, 512], F32)])
            self.sqb = Rot([sc.sb("sqb%d" % i, [128, 512], F32) for i in range(2)])
            self.rstd = sc.sb("rstd", [128, 512], F32)
            xT = sc.sb("xT", [128, FC, 512], F32)
            hTbs = [sc.sb("hTb%d" % i, [128, FC, NSUP], BF16) for i in range(2)]
            w128 = Rot([sc.sb("w128_%d" % i, [128, FC, 128], BF16) for i in range(2)])
            w512 = Rot([sc.sb("w512_%d" % i, [128, FC, 512], BF16) for i in range(1)])
            stgK = Rot([sc.sb("stgK%d" % i, [128, NSUP], BF16) for i in range(2)])
            stgV = Rot([sc.sb("stgV%d" % i, [128, NSUP // 128, 512], BF16) for i in range(1)])
            pmm = Rot([sc.ps("pmm%d" % i, [128, 512], F32) for i in range(4)])
            nsup = SEQ // NSUP
            import os as _os
            kvstage = int(_os.environ.get("KVSTAGE", "9"))
            if kvstage < 9:
                nsup = 1
            ev = 0

            def do_norm(T):
                for q in range(NSUP // 512):
                    self.norm_tile(sc, g["xkv"], NSUP * T + 512 * q, [(128 * i, 128) for i in range(4)], xT, hTbs[T % 2],
                                   512 * q, c["gm1"], 0)

            do_norm(0)
            for T in range(nsup):
                hTb = hTbs[T % 2]
                kch = [(C_KSB + 128 * h, 128, g["KT_sb"], h) for h in range(8)]
                kch += [(C_KDS + 128 * h, 128, g["KT_ds"], h) for h in range(8)]
                kch += [(C_KIX, 64, g["KI"], None)]
                for (c0, ncols, dst, h) in kch:
                    wt = w128.next()
                    self.wload_cols(g["w_in"], c0, ncols, wt)
                    stg = stgK.next()
                    for q in range(NSUP // 512):
                        ps = pmm.next()
                        for fc in range(FC):
                            self.mm(ps, ps[0:ncols, :], wt, wt[:, fc, 0:ncols], hTb, hTb[:, fc, 512 * q:512 * q + 512],
                                    start=(fc == 0), stop=(fc == FC - 1))
                        ev += 1
                        self.cp("act" if ev % 2 else "dve", stg, stg[0:ncols, 512 * q:512 * q + 512], ps, ps[0:ncols, :])
                    if h is None:
                        S.dma("act", dst[:, NSUP * T:NSUP * T + NSUP], stg[0:64, :], reads=[stg], writes=[dst], partial=True)
                    else:
                        S.dma("act", dst[h, :, NSUP * T:NSUP * T + NSUP], stg[:, :], reads=[stg], writes=[dst], partial=True)
                if T + 1 < nsup:
                    do_norm(T + 1)
                for (c0, dst) in ((C_VSB, g["V_sb"]), (C_VDS, g["V_ds"])):
                    for half in range(2):
                        wt = w512.next()
                        for qq in range(4):
                            self.wload_cols(g["w_in"], c0 + 512 * half + 128 * qq, 128, wt, dcol0=128 * qq)
                        stg = stgV.next()
                        for tb in range(NSUP // 128):
                            ps = pmm.next()
                            for fc in range(FC):
                                self.mm(ps, ps[:, :], hTb, hTb[:, fc, 128 * tb:128 * tb + 128], wt, wt[:, fc, :],
                                        start=(fc == 0), stop=(fc == FC - 1))
                            ev += 1
                            self.cp("act" if ev % 2 else "dve", stg, stg[:, tb, :], ps, ps[:, :])
                        kb0 = (NSUP // 128) * T
                        S.dma("act", dst[kb0:kb0 + NSUP // 128, :, 512 * half:512 * half + 512].rearrange("k p n -> p k n"),
                              stg[:, :, :], reads=[stg], writes=[dst], partial=True)

    def query_tile(self, r):
        S, c, g = self.S, self.c, self.g
        halo = r is None
        Nq = 8 if halo else TW
        subs = [(0, 8)] if halo else [(128 * i, 128) for i in range(4)]
        row0 = 2048 if halo else TW * r
        KB = NKB if halo else 16 * (r + 1)
        if self.debug == "short":
            KB = min(KB, 32)
        tname = "h" if halo else "t%d" % r
        with Scope(self, tname) as ts:
            hT = ts.sb("hT", [128, FC, Nq], BF16)
            oTsb = ts.sb("oTsb", [128, 8, Nq], BF16); oTds = ts.sb("oTds", [128, 8, Nq], BF16)
            with Scope(self, tname + "Q") as tq:
                QsbT = tq.sb("QsbT", [128, 8, Nq], BF16); QdsT = tq.sb("QdsT", [128, 8, Nq], BF16)
                QiT = tq.sb("QiT", [128, 8, Nq], BF16); wis = tq.sb("wis", [128, len(subs), 16], F32)
                self.memset("dve", wis, wis[:], 0.0)
                with Scope(self, tname + "A") as sc:
                    self.wst = Rot([sc.sb("wst%d" % i, [128, 2048], F32) for i in range(3)])
                    self.xrow = Rot([sc.sb("xrow%d" % i, [128, D], F32) for i in range(2)])
                    self.ptr = Rot([sc.ps("ptr%d" % i, [128, 512], F32) for i in range(2)])
                    self.pss = Rot([sc.ps("pss", [128, 512], F32)])
                    self.sqb = Rot([sc.sb("sqb%d" % i, [128, 512], F32) for i in range(2)])
                    self.rstd = sc.sb("rstd", [128, 512], F32)
                    xT = sc.sb("xT", [128, FC, Nq], F32)
                    w128 = Rot([sc.sb("w128_%d" % i, [128, FC, 128], BF16) for i in range(3)])
                    pmm = Rot([sc.ps("pmm%d" % i, [128, 512], F32) for i in range(3)])
                    self.norm_tile(sc, g["xq"], row0, subs, xT, hT, 0, c["gm1"], 0)
                    ev = 0
                    for (cbase, dstT) in ((C_QSB, QsbT), (C_QDS, QdsT), (C_QIX, QiT)):
                        for h in range(8):
                            wt = w128.next()
                            self.wload_cols(g["w_in"], cbase + 128 * h, 128, wt)
                            ps = pmm.next()
                            for fc in range(FC):
                                self.mm(ps, ps[:, 0:Nq], wt, wt[:, fc, :], hT, hT[:, fc, :], start=(fc == 0), stop=(fc == FC - 1))
                            ev += 1
                            self.cp("act" if ev % 2 else "dve", dstT, dstT[:, h, :], ps, ps[:, 0:Nq])
                    wt = w128.next()
                    self.wload_cols(g["w_in"], C_WIX, 16, wt)
                    col = 0
                    for si, (ro, nr) in enumerate(subs):
                        ps = pmm.next()
                        for fc in range(FC):
                            self.mm(ps, ps[0:nr, 0:16], hT, hT[:, fc, col:col + nr], wt, wt[:, fc, 0:16],
                                    start=(fc == 0), stop=(fc == FC - 1))
                        self.act(wis, wis[0:nr, si, :], ps, ps[0:nr, 0:16], AF.Copy, scale=WIS_SCALE)
                        col += nr
                if self.debug:
                    self.dump("dbg_hT_" + tname, hT, hT[:].rearrange("p a b -> p (a b)"), [128, FC * Nq], BF16)
                    self.dump("dbg_QiT_" + tname, QiT, QiT[:].rearrange("p a b -> p (a b)"), [128, 8 * Nq], BF16)
                    self.dump("dbg_wis_" + tname, wis, wis[:].rearrange("p a b -> p (a b)"), [128, len(subs) * 16], F32)
                self.check_stop(tname + "A")
                self.sb_attention(tname, r, Nq, KB, QsbT, oTsb)
                if self.debug:
                    self.dump("dbg_oTsb_" + tname, oTsb, oTsb[:].rearrange("p a b -> p (a b)"), [128, 8 * Nq], BF16)
                self.check_stop(tname + "B")
                self.indexer(tname, r, Nq, subs, KB, QiT, wis)
                self.check_stop(tname + "C")
                self.dsa_attention(tname, r, Nq, KB, QdsT, oTds)
                if self.debug:
                    self.dump("dbg_oTds_" + tname, oTds, oTds[:].rearrange("p a b -> p (a b)"), [128, 8 * Nq], BF16)
                self.check_stop(tname + "D")
            self.post_attention(tname, r, Nq, subs, row0, hT, oTsb, oTds)

    def dump(self, name, t, ap, shape, dt):
        d = self.dscr(name, shape, dt)
        self.S.dma("sp", d[:, :], ap, reads=[t], writes=[d])

    def kv_chunk_load(self, KTd, Vd, h, cidx, KTc, Vc):
        S = self.S
        S.dma("sp", KTc[:, :], KTd[h, :, 2048 * cidx:2048 * cidx + 2048], reads=[KTd], writes=[KTc])
        S.dma("sp", Vc[:, :, :], Vd[16 * cidx:16 * cidx + 16, :, 128 * h:128 * h + 128].rearrange("k p n -> p k n"),
              reads=[Vd], writes=[Vc])

    def sb_attention(self, tname, r, Nq, KB, QsbT, oTsb):
        S, c, g = self.S, self.c, self.g
        halo = r is None
        with Scope(self, tname + "B") as sc:
            KTc = Rot([sc.sb("KTc%d" % i, [128, 2048], BF16) for i in range(3)])
            Vc = Rot([sc.sb("Vc%d" % i, [128, 16, 128], BF16) for i in range(3)])
            e32 = Rot([sc.sb("e32_%d" % i, [128, Nq], F32) for i in range(7)])
            spb = Rot([sc.sb("spb%d" % i, [128, Nq], BF16) for i in range(5)])
            w32 = Rot([sc.sb("w32_%d" % i, [128, Nq], F32) for i in range(3)])
            ab = Rot([sc.sb("ab%d" % i, [128, Nq], BF16) for i in range(3)])
            Rbf = Rot([sc.sb("Rbf%d" % i, [128, Nq], BF16) for i in range(4)])
            LT = Rot([sc.ps("LT%d" % i, [128, 512], F32) for i in range(3)])
            CP = Rot([sc.ps("CP%d" % i, [128, 512], F32) for i in range(3)])
            ACC = Rot([sc.ps("ACC%d" % i, [128, 512], F32) for i in range(2)])
            steps = []
            for h in range(8):
                for kb in range(KB - 1, -1, -1):
                    steps.append(dict(h=h, kb=kb, first=(kb == KB - 1), last=(kb == 0)))
            st = dict(ktc=None, vc=None, acc=None, rb=None)

            def g0(d):
                kb, h = d["kb"], d["h"]
                kbl = kb % 16
                if kbl == 15 or d["first"]:
                    st["ktc"] = KTc.next(); st["vc"] = Vc.next()
                    self.kv_chunk_load(g["KT_sb"], g["V_sb"], h, kb // 16, st["ktc"], st["vc"])
                if d["first"]:
                    st["acc"] = ACC.next()
                d["vc"] = st["vc"]; d["acc"] = st["acc"]
                ktc = st["ktc"]
                lt = LT.next()
                d["lt"] = lt
                self.mm(lt, lt[:, 0:Nq], ktc, ktc[:, 128 * kbl:128 * kbl + 128], QsbT, QsbT[:, h, :], True, True)

            def g1(d):
                kb = d["kb"]
                lt = d["lt"]
                e = e32.next()
                d["e"] = e
                self.act(e, e[:, :], lt, lt[:, 0:Nq], AF.Exp, scale=SCALE)
                if halo:
                    self.tt("dve", e, e[:, :], e, e[:, :], c["FHc"], c["FHc"][:, 63 - kb, :], ALU.mult)
                else:
                    kk = kb - 16 * r + 1
                    if 0 <= kk <= 16:
                        o0 = 2 + 128 * (16 - kk)
                        self.tt("dve", e, e[:, :], e, e[:, :], c["wc"], c["wc"][:, o0:o0 + Nq], ALU.mult)

            def g2(d):
                e = d["e"]
                sp = spb.next()
                d["sp"] = sp
                self.act(sp, sp[:, :], e, e[:, :], AF.Ln, bias=1.0)

            def g3(d):
                sp = d["sp"]
                cp_ = CP.next()
                d["cp"] = cp_
                first = d["first"]
                self.mm(cp_, cp_[:, 0:Nq], c["trib"], c["trib"][:, :], sp, sp[:, :], True, first)
                if not first:
                    rb = st["rb"]
                    self.mm(cp_, cp_[:, 0:Nq], c["onesb"], c["onesb"][:, :], rb, rb[:, :], False, True)
                if not d["last"]:
                    if first:
                        st["rb"] = sp
                    else:
                        rn = Rbf.next()
                        self.tt("dve", rn, rn[:, :], st["rb"], st["rb"][:, :], sp, sp[:, :], ALU.add)
                        st["rb"] = rn

            def g4(d):
                cp_ = d["cp"]
                w = w32.next()
                d["w"] = w
                self.act(w, w[:, :], cp_, cp_[:, 0:Nq], AF.Exp, scale=-1.0)

            def g5(d):
                e, w = d["e"], d["w"]
                a = ab.next()
                d["a"] = a
                self.tt("dve", a, a[:, :], e, e[:, :], w, w[:, :], ALU.mult)

            def g6(d):
                a, acc, vc = d["a"], d["acc"], d["vc"]
                self.mm(acc, acc[:, 0:Nq], vc, vc[:, d["kb"] % 16, :], a, a[:, :], d["first"], d["last"])
                if d["last"]:
                    self.cp("act", oTsb, oTsb[:, d["h"], :], acc, acc[:, 0:Nq])

            self.pipeline(steps, [g0, g1, g2, g3, g4, g5, g6])

    def pipeline(self, steps, stages):
        n = len(steps)
        ns = len(stages)
        for i in range(n + ns - 1):
            for k, fn in enumerate(stages):
                j = i - k
                if 0 <= j < n:
                    fn(steps[j])

    def indexer(self, tname, r, Nq, subs, KB, QiT, wis):
        S, c, g = self.S, self.c, self.g
        halo = r is None
        Lk = 128 * KB
        with Scope(self, tname + "C") as sc:
            KI2 = sc.sb("KI2", [128, Lk], BF16)
            scores = Rot([sc.sb("score%d" % i, [128, Lk], F32) for i in range(2)])
            maskb = sc.sb("maskb", [128, Lk], BF16)
            m8 = sc.sb("m8", [128, 8], F32)
            rl = Rot([sc.sb("rl%d" % i, [128, 512], BF16) for i in range(4)])
            Dg = Rot([sc.sb("Dg%d" % i, [128, 16, 128], BF16) for i in range(2)])
            mstg = Rot([sc.sb("mstg%d" % i, [128, 8, 128], BF16) for i in range(2)])
            SC = Rot([sc.ps("SC%d" % i, [128, 512], F32) for i in range(4)])
            SA = Rot([sc.ps("SA%d" % i, [128, 512], F32) for i in range(2)])
            TP = Rot([sc.ps("TP%d" % i, [128, 1024], BF16) for i in range(2)])
            if halo:
                cap = sc.sb("hcap", [8, Lk], F32)
                S.dma("sp", cap[:, :], g["hcapd"][:, 0:Lk], reads=[g["hcapd"]], writes=[cap])
            else:
                cap = sc.sb("wcap", [128, WCW], F32)
                S.dma("sp", cap[:, :], g["wcapd"][:, :], reads=[g["wcapd"]], writes=[cap])
            S.dma("sp", KI2[0:64, :], g["KI"][:, 0:Lk], reads=[g["KI"]], writes=[KI2], partial=True)
            S.dma("sp", KI2[64:128, :], g["KI"][:, 0:Lk], reads=[g["KI"]], writes=[KI2], partial=True)
            cols = []
            col = 0
            for (ro, nr) in subs:
                cols.append(col)
                col += nr
            sstate = {}

            def idx_phase(si):
                ro, nr = subs[si]
                col = cols[si]
                score = scores.next()
                sstate[si] = score
                dg = Dg.next()
                for hi in range(16):
                    self.ts("dve", dg, dg[0:nr, hi, 0:nr], c["identb"], c["identb"][0:nr, 0:nr], wis[0:nr, si, hi:hi + 1],
                            ALU.mult, extra_reads=[wis])
                steps = [dict(ch=ch, hi=hi) for ch in range(Lk // 512) for hi in range(16)]
                cur = {}

                def s1(d):
                    hi, ch = d["hi"], d["ch"]
                    b0 = 64 * (hi % 2)
                    ps = SC.next()
                    d["ps"] = ps
                    self.mm(ps, ps[0:nr, :], QiT, QiT[b0:b0 + 64, hi // 2, col:col + nr],
                            KI2, KI2[b0:b0 + 64, 512 * ch:512 * ch + 512], True, True)

                def s2(d):
                    t = rl.next()
                    d["t"] = t
                    self.act(t, t[0:nr, :], d["ps"], d["ps"][0:nr, :], AF.Relu)

                def s3(d):
                    hi, ch = d["hi"], d["ch"]
                    if hi == 0:
                        cur["sa"] = SA.next()
                    sa = cur["sa"]
                    t = d["t"]
                    self.mm(sa, sa[0:nr, :], dg, dg[0:nr, hi, 0:nr], t, t[0:nr, :], hi == 0, hi == 15)
                    if hi == 15:
                        self.cp("act", score, score[0:nr, 512 * ch:512 * ch + 512], sa, sa[0:nr, :])

                self.pipeline(steps, [s1, s2, s3])

            def topk_phase(si):
                ro, nr = subs[si]
                col = cols[si]
                score = sstate[si]
                if halo:
                    self.tt("dve", score, score[0:nr, :], score, score[0:nr, :], cap, cap[0:nr, 0:Lk], ALU.min)
                else:
                    for kk in range(17):
                        kb = 16 * r - 1 + kk
                        if kb < 0 or kb >= KB:
                            continue
                        o0 = 2 + 128 * (16 - kk + si)
                        self.tt("dve", score, score[0:nr, 128 * kb:128 * kb + 128], score, score[0:nr, 128 * kb:128 * kb + 128],
                                cap, cap[0:nr, o0:o0 + 128], ALU.min)
                for it in range(32):
                    S.op("dve", lambda e: e.max(out=m8[0:nr, :], in_=score[0:nr, :]), reads=[score], writes=[m8])
                    S.op("dve", lambda e: e.match_replace(out=score[0:nr, :], in_to_replace=m8[0:nr, :],
                                                          in_values=score[0:nr, :], imm_value=NEG),
                         reads=[score, m8], writes=[score])
                self.ts("dve", maskb, maskb[0:nr, :], score, score[0:nr, :], -2.0e38, ALU.is_le)
                for kb0 in range(0, KB, 8):
                    tp = TP.next()
                    for i in range(8):
                        kb = kb0 + i
                        self.tr(tp, tp[:, 128 * i:128 * i + nr], maskb, maskb[0:nr, 128 * kb:128 * kb + 128],
                                c["identb"], c["identb"][0:nr, 0:nr])
                    ms = mstg.next()
                    self.cp("act", ms, ms[:, :, 0:nr], tp, tp[:, :].rearrange("p (i n) -> p i n", n=128)[:, :, 0:nr])
                    S.dma("act", g["mTd"][kb0:kb0 + 8, :, col:col + nr].rearrange("k p n -> p k n"), ms[:, :, 0:nr],
                          reads=[ms], writes=[g["mTd"]], partial=True)

            nsub = len(subs)
            idx_phase(0)
            for si in range(nsub):
                if si + 1 < nsub:
                    idx_phase(si + 1)
                topk_phase(si)

    def dsa_attention(self, tname, r, Nq, KB, QdsT, oTds):
        S, c, g = self.S, self.c, self.g
        halo = r is None
        with Scope(self, tname + "D") as sc:
            KTc = Rot([sc.sb("KTc%d" % i, [128, 2048], BF16) for i in range(2)])
            Vc = Rot([sc.sb("Vc%d" % i, [128, 16, 128], BF16) for i in range(2)])
            mTc = Rot([sc.sb("mTc%d" % i, [128, 16, Nq], BF16) for i in range(2)])
            Wd = Rot([sc.sb("Wd%d" % i, [128, WCW], BF16) for i in range(2)])
            pb = Rot([sc.sb("pb%d" % i, [128, Nq], BF16) for i in range(4)])
            p2 = Rot([sc.sb("p2_%d" % i, [128, Nq], BF16) for i in range(4)])
            dens = sc.sb("dens", [128, Nq], F32)
            LT = Rot([sc.ps("LT%d" % i, [128, 512], F32) for i in range(3)])
            ACC = Rot([sc.ps("ACC%d" % i, [128, 512], F32) for i in range(2)])
            DEN = Rot([sc.ps("DEN%d" % i, [128, 512], F32) for i in range(2)])
            steps = []
            for h in range(8):
                for kb in range(KB):
                    steps.append(dict(h=h, kb=kb, first=(kb == 0), last=(kb == KB - 1)))
            st = dict(ktc=None, vc=None, mtc=None, acc=None, den=None, wd=None)

            def g0(d):
                h, kb = d["h"], d["kb"]
                kbl = kb % 16
                if d["first"]:
                    st["acc"] = ACC.next(); st["den"] = DEN.next()
                    if not halo:
                        wd = Wd.next()
                        st["wd"] = wd
                        S.dma("sp", wd[:, :], bass.AP(g["gv"].t.tensor, h * LGV, [[1, 128], [1, WCW]]),
                              reads=[g["gv"]], writes=[wd])
                if kbl == 0:
                    st["ktc"] = KTc.next(); st["vc"] = Vc.next(); st["mtc"] = mTc.next()
                    self.kv_chunk_load(g["KT_ds"], g["V_ds"], h, kb // 16, st["ktc"], st["vc"])
                    S.dma("sp", st["mtc"][:, :, :], g["mTd"][kb:kb + 16, :, 0:Nq].rearrange("k p n -> p k n"),
                          reads=[g["mTd"]], writes=[st["mtc"]])
                for k_ in ("vc", "mtc", "acc", "den", "wd"):
                    d[k_] = st[k_]
                ktc = st["ktc"]
                lt = LT.next()
                d["lt"] = lt
                self.mm(lt, lt[:, 0:Nq], ktc, ktc[:, 128 * kbl:128 * kbl + 128], QdsT, QdsT[:, h, :], True, True)

            def g1(d):
                lt = d["lt"]
                p = pb.next()
                d["p"] = p
                self.act(p, p[:, :], lt, lt[:, 0:Nq], AF.Exp, scale=SCALE)

            def g2(d):
                h, kb = d["h"], d["kb"]
                kbl = kb % 16
                p, mtc, wd = d["p"], d["mtc"], d["wd"]
                q2 = p2.next()
                d["q2"] = q2
                self.tt("dve", q2, q2[:, :], p, p[:, :], mtc, mtc[:, kbl, :], ALU.mult)
                if halo:
                    fh = c["FHd"][:, h, :].rearrange("p (k e) -> p k e", e=8)[:, 63 - kb, :]
                    self.tt("dve", q2, q2[:, :], q2, q2[:, :], c["FHd"], fh, ALU.mult)
                else:
                    kk = kb - 16 * r + 1
                    if 0 <= kk <= 16:
                        o0 = 2 + 128 * (16 - kk)
                        self.tt("dve", q2, q2[:, :], q2, q2[:, :], wd, wd[:, o0:o0 + Nq], ALU.mult)

            def g3(d):
                h, kb = d["h"], d["kb"]
                kbl = kb % 16
                q2, vc, acc, den = d["q2"], d["vc"], d["acc"], d["den"]
                self.mm(acc, acc[:, 0:Nq], vc, vc[:, kbl, :], q2, q2[:, :], d["first"], d["last"])
                self.mm(den, den[:, 0:Nq], c["onesb"], c["onesb"][:, :], q2, q2[:, :], d["first"], d["last"])
                if d["last"]:
                    self.ts("dve", dens, dens[:, :], den, den[:, 0:Nq], 1e-30, ALU.max)
                    S.op("dve", lambda e: e.reciprocal(out=dens[:, :], in_=dens[:, :]), reads=[dens], writes=[dens])
                    self.tt("dve", oTds, oTds[:, h, :], acc, acc[:, 0:Nq], dens, dens[:, :], ALU.mult)

            self.pipeline(steps, [g0, g1, g2, g3])

    def post_attention(self, tname, r, Nq, subs, row0, hT, oTsb, oTds):
        S, c, g = self.S, self.c, self.g
        halo = r is None
        m = c["modT"]
        with Scope(self, tname + "E") as se:
            xT = se.sb("xT", [128, FC, Nq], F32)
            self.wst = Rot([se.sb("wst%d" % i, [128, 2048], F32) for i in range(3)])
            self.sqb = Rot([se.sb("sqb%d" % i, [128, 512], F32) for i in range(2)])
            self.rstd = se.sb("rstd", [128, 512], F32)
            with Scope(self, tname + "E1") as sc:
                self.xrow = Rot([sc.sb("xrow%d" % i, [128, D], F32) for i in range(2)])
                self.ptr = Rot([sc.ps("ptr%d" % i, [128, 512], F32) for i in range(2)])
                merged = sc.sb("merged", [128, FC, Nq], BF16)
                wo = Rot([sc.sb("wo%d" % i, [128, 8, 128], BF16) for i in range(2)])
                w128 = Rot([sc.sb("w128_%d" % i, [128, FC, 128], BF16) for i in range(3)])
                sg = Rot([sc.sb("sg%d" % i, [128, Nq], F32) for i in range(2)])
                m1 = Rot([sc.sb("m1_%d" % i, [128, Nq], F32) for i in range(2)])
                m2 = Rot([sc.sb("m2_%d" % i, [128, Nq], F32) for i in range(2)])
                PA = Rot([sc.ps("PA%d" % i, [128, 512], F32) for i in range(2)])
                PG = Rot([sc.ps("PG%d" % i, [128, 512], F32) for i in range(2)])
                self.norm_tile(sc, g["xq"], row0, subs, xT, None, 0, None, 0, do_norm=False)
                for cc in range(FC):
                    parts = []
                    for (wo_d, oT, gcol, mm_) in ((g["w_o_sb"], oTsb, C_GSB, m1), (g["w_o_dsa"], oTds, C_GDS, m2)):
                        wt = wo.next()
                        self.wload_cols(wo_d, 128 * cc, 128, wt, nk=8)
                        pa = PA.next()
                        for h in range(8):
                            self.mm(pa, pa[:, 0:Nq], wt, wt[:, h, :], oT, oT[:, h, :], h == 0, h == 7)
                        wg = w128.next()
                        self.wload_cols(g["w_in"], gcol + 128 * cc, 128, wg)
                        pg = PG.next()
                        for fc in range(FC):
                            self.mm(pg, pg[:, 0:Nq], wg, wg[:, fc, :], hT, hT[:, fc, :], fc == 0, fc == FC - 1)
                        s_ = sg.next()
                        self.act(s_, s_[:, :], pg, pg[:, 0:Nq], AF.Sigmoid)
                        mt = mm_.next()
                        self.tt("dve", mt, mt[:, :], pa, pa[:, 0:Nq], s_, s_[:, :], ALU.mult)
                        parts.append(mt)
                    self.tt("pool", merged, merged[:, cc, :], parts[0], parts[0][:, :], parts[1], parts[1][:, :], ALU.add)
                for cc in range(FC):
                    wt = w128.next()
                    self.wload_cols(g["w_out"], 128 * cc, 128, wt)
                    pa = PA.next()
                    for k in range(FC):
                        self.mm(pa, pa[:, 0:Nq], wt, wt[:, k, :], merged, merged[:, k, :], k == 0, k == FC - 1)
                    self.stt(xT, xT[:, cc, :], pa, pa[:, 0:Nq], m[:, 32 + cc:33 + cc], xT, xT[:, cc, :], ALU.mult, ALU.add,
                             extra_reads=[m])
            if self.debug:
                self.dump("dbg_xpost_" + tname, xT, xT[:].rearrange("p a b -> p (a b)"), [128, FC * Nq], F32)
            with Scope(self, tname + "E2") as sc:
                self.pss = Rot([sc.ps("pss", [128, 512], F32)])
                PA = Rot([sc.ps("PA%d" % i, [128, 512], F32) for i in range(2)])
                PU = Rot([sc.ps("PU%d" % i, [128, 512], F32) for i in range(2)])
                self.norm_from_xT(sc, xT, Nq, hT, 0, c["gm2"], 48)
                uT = None if halo else sc.sb("uT", [128, NFF, Nq], BF16)
                with Scope(self, tname + "E2a") as sa:
                    w128 = Rot([sa.sb("w128_%d" % i, [128, FC, 128], BF16) for i in range(4)])
                    if not halo:
                        aext = Rot([sa.sb("aext%d" % i, [128, Nq + 2], F32) for i in range(2)])
                        yb = Rot([sa.sb("yb%d" % i, [128, Nq], F32) for i in range(2)])
                        sl = Rot([sa.sb("sl%d" % i, [128, Nq], F32) for i in range(2)])
                    for k in range(NFF):
                        wg = w128.next()
                        self.wload_cols(g["w_gate"], 128 * k, 128, wg)
                        pa = PA.next()
                        for fc in range(FC):
                            self.mm(pa, pa[:, 0:Nq], wg, wg[:, fc, :], hT, hT[:, fc, :], fc == 0, fc == FC - 1)
                        if halo:
                            self.tt("dve", c["a_halo"], c["a_halo"][:, k, :], pa, pa[:, 0:Nq], c["hv"], c["hv"][:, :], ALU.mult)
                            continue
                        wu = w128.next()
                        self.wload_cols(g["w_up"], 128 * k, 128, wu)
                        pu = PU.next()
                        for fc in range(FC):
                            self.mm(pu, pu[:, 0:Nq], wu, wu[:, fc, :], hT, hT[:, fc, :], fc == 0, fc == FC - 1)
                        ae = aext.next()
                        self.cp("act", ae, ae[:, 2:Nq + 2], pa, pa[:, 0:Nq])
                        self.cp("pool", ae, ae[:, 0:2], c["a_halo"], c["a_halo"][:, k, 2 * r:2 * r + 2])
                        y = yb.next()
                        cwk = c["cw"]
                        self.ts("dve", y, y[:, :], ae, ae[:, 0:Nq], cwk[:, 3 * k:3 * k + 1], ALU.mult,
                                c["cb"][:, k:k + 1], ALU.add, extra_reads=[cwk, c["cb"]])
                        self.stt(y, y[:, :], ae, ae[:, 1:Nq + 1], cwk[:, 3 * k + 1:3 * k + 2], y, y[:, :], ALU.mult, ALU.add,
                                 extra_reads=[cwk])
                        self.stt(y, y[:, :], ae, ae[:, 2:Nq + 2], cwk[:, 3 * k + 2:3 * k + 3], y, y[:, :], ALU.mult, ALU.add,
                                 extra_reads=[cwk])
                        s_ = sl.next()
                        self.act(s_, s_[:, :], y, y[:, :], AF.Silu)
                        self.tt("dve", uT, uT[:, k, :], pu, pu[:, 0:Nq], s_, s_[:, :], ALU.mult)
                if halo:
                    if self.debug:
                        self.dump("dbg_ahalo", c["a_halo"], c["a_halo"][:].rearrange("p a b -> p (a b)"), [128, NFF * 8], F32)
                    return
                with Scope(self, tname + "E2b") as sb_:
                    wdn = Rot([sb_.sb("wdn%d" % i, [128, NFF, 128], BF16) for i in range(2)])
                    for cc in range(FC):
                        wt = wdn.next()
                        self.wload_cols(g["w_down"], 128 * cc, 128, wt, nk=NFF)
                        pa = PA.next()
                        for k in range(NFF):
                            self.mm(pa, pa[:, 0:Nq], wt, wt[:, k, :], uT, uT[:, k, :], k == 0, k == NFF - 1)
                        self.stt(xT, xT[:, cc, :], pa, pa[:, 0:Nq], m[:, 80 + cc:81 + cc], xT, xT[:, cc, :], ALU.mult, ALU.add,
                                 extra_reads=[m])
                    rs = self.norm_stats(xT, Nq)
                    for cc in range(FC):
                        self.stt(xT, xT[:, cc, :], xT, xT[:, cc, :], c["gfin"][:, cc:cc + 1], rs, rs[:, 0:Nq], ALU.mult, ALU.mult,
                                 extra_reads=[c["gfin"]])
                    orow = Rot([sb_.sb("orow%d" % i, [128, D], F32) for i in range(2)])
                    PO = Rot([sb_.ps("PO%d" % i, [128, 512], F32) for i in range(2)])
                    ev = 0
                    for tb in range(4):
                        o = orow.next()
                        for gq in range(4):
                            po = PO.next()
                            for i in range(4):
                                cc = 4 * gq + i
                                self.tr(po, po[:, 128 * i:128 * i + 128], xT, xT[:, cc, 128 * tb:128 * tb + 128],
                                        c["ident"], c["ident"][:, :])
                            ev += 1
                            self.cp("act" if ev % 2 else "dve", o, o[:, 512 * gq:512 * gq + 512], po, po[:, :])
                        S.dma("sp", g["yout"][row0 + 128 * tb:row0 + 128 * tb + 128, :], o[:, :], reads=[o], writes=[g["yout"]],
                              partial=True)


def _bucket(delta):
    n = np.maximum(delta, 0)
    nf = np.maximum(n, 1).astype(np.float32)
    large = 16 + (np.log(nf / np.float32(16)) / np.float32(math.log(8.0)) * np.float32(16)).astype(np.int32)
    large = np.minimum(large, 31)
    return np.where(n < 16, n, large)


def _colT(v, k):
    return np.ascontiguousarray(np.asarray(v, np.float32).reshape(k, 128).T)


def make_core_inputs(inp, b, j):
    x = np.asarray(inp["x"], np.float32)
    xb = x[b]
    xq = np.zeros((2056, D), np.float32)
    for r in range(NTILE):
        t0 = TW * (4 * r + j)
        xq[TW * r:TW * r + TW] = xb[t0:t0 + TW]
        for e in range(2):
            t = t0 - 2 + e
            if t >= 0:
                xq[2048 + 2 * r + e] = xb[t]
    xkv = np.ascontiguousarray(xb.reshape(NKB, 128, D)[:, ::-1, :].reshape(SEQ, D))
    p = np.arange(128)[:, None]
    u = np.arange(WCW)[None, :]
    delta = (u - 2) + p + 512 * j - 2047
    wc = (delta >= 1).astype(np.float32)
    wcap = np.where(delta >= 0, BIG, -BIG).astype(np.float32)
    mm_ = np.arange(LGV)
    dm = mm_ - 2 + 512 * j - 2047
    ohb = np.zeros((32, LGV), np.float32)
    bk = _bucket(dm)
    valid = dm >= 0
    ohb[bk[valid], mm_[valid]] = 1.0
    cz = np.broadcast_to(valid.astype(np.float32)[None, :], (8, LGV)).copy()
    hcap = np.zeros((8, SEQ), np.float32)
    hv = np.ones((128, 8), np.float32)
    kbs = np.arange(NKB)[:, None]
    pp = np.arange(128)[None, :]
    for r in range(NTILE):
        for e in range(2):
            tpos = TW * (4 * r + j) - 2 + e
            spos = 128 * kbs + 127 - pp
            hcap[2 * r + e] = np.where(tpos - spos >= 0, BIG, -BIG).reshape(-1)
            if tpos < 0:
                hv[:, 2 * r + e] = 0.0
    d = {
        "xq": xq, "xkv": xkv, "cT": _colT(inp["c"][b], 16),
        "w_ada": np.ascontiguousarray(np.asarray(inp["w_ada"], np.float32)[0]),
        "b_adaT": _colT(np.asarray(inp["b_ada"])[0], 96),
        "g_mixT": _colT(np.asarray(inp["g_mix"])[0], 16), "g_ffnT": _colT(np.asarray(inp["g_ffn"])[0], 16),
        "g_finT": _colT(np.asarray(inp["g_final"]), 16),
        "w_in": np.ascontiguousarray(np.asarray(inp["w_in"], np.float32)[0]),
        "w_o_sb": np.ascontiguousarray(np.asarray(inp["w_o_sb"], np.float32)[0]),
        "w_o_dsa": np.ascontiguousarray(np.asarray(inp["w_o_dsa"], np.float32)[0]),
        "w_out": np.ascontiguousarray(np.asarray(inp["w_out"], np.float32)[0]),
        "rel_bias": np.ascontiguousarray(np.asarray(inp["rel_bias"], np.float32)),
        "w_gate": np.ascontiguousarray(np.asarray(inp["w_gate"], np.float32)[0]),
        "w_up": np.ascontiguousarray(np.asarray(inp["w_up"], np.float32)[0]),
        "convT": np.ascontiguousarray(np.asarray(inp["conv_w"], np.float32)[0].reshape(3, NFF, 128).transpose(2, 1, 0).reshape(128, NFF * 3)),
        "convb": _colT(np.asarray(inp["conv_b"])[0], NFF),
        "w_down": np.ascontiguousarray(np.asarray(inp["w_down"], np.float32)[0]),
        "ident": np.eye(128, dtype=np.float32),
        "tri": (np.arange(128)[:, None] <= np.arange(128)[None, :]).astype(np.float32),
        "wc": wc, "wcap": wcap, "hcap": hcap, "hv": hv, "ohb": ohb, "cz": cz,
    }
    return d


_NC_CACHE = {}


def kernel(**inputs):
    if "nc" not in _NC_CACHE:
        _NC_CACHE["nc"] = Builder().build()
    nc = _NC_CACHE["nc"]
    in_maps = []
    for core in range(8):
        b, j = core // 4, core % 4
        in_maps.append(make_core_inputs(inputs, b, j))
    res = run_bass_kernel_spmd(nc, in_maps, core_ids=list(range(8)))
    out = np.zeros((2, SEQ, D), np.float32)
    for core in range(8):
        b, j = core // 4, core % 4
        y = np.asarray(res.results[core]["y"], np.float32)
        for r in range(NTILE):
            t0 = TW * (4 * r + j)
            out[b, t0:t0 + TW] = y[TW * r:TW * r + TW]
    return out
```

```python
import math
import contextlib
import numpy as np
import concourse.bass as bass
import concourse.mybir as mybir
from concourse.bass_utils import run_bass_kernel_spmd

F32 = mybir.dt.float32
BF16 = mybir.dt.bfloat16
AF = mybir.ActivationFunctionType
ALU = mybir.AluOpType

D = 2048
SEQ = 8192
FC = 16
DFF = 5632
NFF = 44
PIN = 11344
NTILE = 4
TW = 512
WCW = 2562
LGV = 2690
NKB = 64
SCALE = 128.0 ** -0.5
WIS_SCALE = (16.0 ** -0.5) * (64.0 ** -0.5)
NEG = -3.0e38
BIG = 1.0e30

C_QSB, C_KSB, C_VSB, C_QDS, C_KDS, C_VDS, C_QIX, C_KIX, C_WIX, C_GSB, C_GDS = (
    0, 1024, 2048, 3072, 4096, 5120, 6144, 7168, 7232, 7248, 9296)


class DSem:
    def __init__(self, h):
        self.h = h
        self.count = 0


class Tk:
    def __init__(self, t, name):
        self.t = t
        self.name = name
        self.wev = {}
        self.readers = {}

    def __getitem__(self, idx):
        return self.t[idx]


NRING = 40


class Sched:
    def __init__(self, nc):
        self.nc = nc
        self.eng = {"pe": nc.tensor, "act": nc.scalar, "dve": nc.vector, "pool": nc.gpsimd, "sp": nc.sync}
        self.sem = {k: nc.alloc_semaphore(name="sem_" + k) for k in ("pe", "act", "dve", "pool")}
        self.cnt = {k: 0 for k in self.sem}
        self.seen = {k: {} for k in self.eng}
        self.ring = [DSem(nc.alloc_semaphore(name="dsem%d" % i)) for i in range(NRING)]
        self.all_dsems = self.ring
        self.ri = 0
        self.n_inst = 0
        self.n_wait = 0

    def release(self, tiles):
        pass

    def _wait(self, eng, deps):
        e = self.eng[eng]
        for key, val in deps:
            if key == "pe" and eng == "pe":
                continue
            if self.seen[eng].get(key, 0) >= val:
                continue
            sem = self.sem[key] if isinstance(key, str) else key.h
            e.wait_ge(sem, val)
            self.n_wait += 1
            self.seen[eng][key] = val

    @staticmethod
    def _deps(reads, writes, partial=False):
        deps = []
        for t in reads:
            deps.extend(t.wev.items())
        for t in writes:
            for k, v in t.wev.items():
                if partial and not isinstance(k, str):
                    continue
                deps.append((k, v))
            deps.extend(t.readers.items())
        return deps

    def op(self, eng, fn, reads=(), writes=()):
        self._wait(eng, self._deps(reads, writes))
        inst = fn(self.eng[eng])
        self.cnt[eng] += 1
        self.n_inst += 1
        inst.then_inc(self.sem[eng], 1)
        val = self.cnt[eng]
        for t in reads:
            t.readers[eng] = val
        for t in writes:
            t.wev = {eng: val}
            t.readers = {}

    def dma(self, q, out_ap, in_ap, reads=(), writes=(), partial=False):
        dst = writes[0]
        ds = self.ring[self.ri % NRING]
        self.ri += 1
        deps = self._deps(reads, writes, partial)
        if ds.count > 0:
            deps.append((ds, ds.count))
        self._wait(q, deps)
        inst = self.eng[q].dma_start(out=out_ap, in_=in_ap)
        inst.then_inc(ds.h, 16)
        ds.count += 16
        self.n_inst += 1
        for t in reads:
            t.readers[ds] = ds.count
        if partial:
            dst.wev[ds] = ds.count
        else:
            dst.wev = {ds: ds.count}
        dst.readers = {}

    def barrier(self, engines=None):
        deps = [(k, self.cnt[k]) for k in self.sem if self.cnt[k] > 0]
        deps += [(ds, ds.count) for ds in self.all_dsems if ds.count > 0]
        for eng in (engines or self.eng):
            self._wait(eng, deps)


class Scope:
    def __init__(self, K, name):
        self.K = K
        self.name = name
        self.es = contextlib.ExitStack()
        self.tiles = []

    def sb(self, name, shape, dt):
        t = Tk(self.es.enter_context(self.K.nc.sbuf_tensor(self.name + "_" + name, list(shape), dt)), name)
        self.tiles.append(t)
        return t

    def ps(self, name, shape, dt=F32):
        t = Tk(self.es.enter_context(self.K.nc.psum_tensor(self.name + "_" + name, list(shape), dt)), name)
        self.tiles.append(t)
        return t

    def __enter__(self):
        return self

    def __exit__(self, *a):
        if a[0] is None or a[0] is StopBuild:
            self.K.S.barrier()
        self.es.close()
        return False


def sub_ap(ap, off, dims):
    return bass.AP(ap.tensor, ap.offset + off, [list(ap.ap[0])] + [list(d) for d in dims])


class StopBuild(Exception):
    pass


class Rot:
    def __init__(self, tiles):
        self.tiles = tiles
        self.i = 0

    def next(self):
        t = self.tiles[self.i % len(self.tiles)]
        self.i += 1
        return t


class Builder:
    def __init__(self, stop_after=None, debug=False):
        self.stop_after = stop_after
        self.debug = debug
        self.nc = bass.Bass("TRN2", target_bir_lowering=False)
        self.S = Sched(self.nc)
        self.dr = {}

    def din(self, name, shape):
        t = self.nc.dram_tensor(name, list(shape), F32, kind="ExternalInput")
        self.dr[name] = Tk(t.ap(), name)
        return self.dr[name]

    def dscr(self, name, shape, dt):
        kind = "ExternalOutput" if self.debug else "Internal"
        t = self.nc.dram_tensor(name, list(shape), dt, kind=kind)
        self.dr[name] = Tk(t.ap(), name)
        return self.dr[name]

    def mm(self, ot, o, lt, l, rt, r, start, stop):
        self.S.op("pe", lambda e: e.matmul(o, lhsT=l, rhs=r, start=start, stop=stop), reads=[lt, rt], writes=[ot])

    def tr(self, ot, o, it, i, idt, ident):
        self.S.op("pe", lambda e: e.transpose(o, i, ident), reads=[it, idt], writes=[ot])

    def act(self, ot, o, it, i, func, scale=1.0, bias=None, extra_reads=()):
        kw = {}
        if bias is not None:
            kw["bias"] = bias
        self.S.op("act", lambda e: e.activation(out=o, in_=i, func=func, scale=scale, **kw),
                  reads=[it] + list(extra_reads), writes=[ot])

    def tt(self, eng, ot, o, at, a, bt, b, op):
        self.S.op(eng, lambda e: e.tensor_tensor(out=o, in0=a, in1=b, op=op), reads=[at, bt], writes=[ot])

    def ts(self, eng, ot, o, it, i, s1, op0, s2=None, op1=None, extra_reads=()):
        if op1 is None:
            self.S.op(eng, lambda e: e.tensor_scalar(out=o, in0=i, scalar1=s1, scalar2=None, op0=op0),
                      reads=[it] + list(extra_reads), writes=[ot])
        else:
            self.S.op(eng, lambda e: e.tensor_scalar(out=o, in0=i, scalar1=s1, scalar2=s2, op0=op0, op1=op1),
                      reads=[it] + list(extra_reads), writes=[ot])

    def stt(self, ot, o, at, a, scalar, bt, b, op0, op1, extra_reads=()):
        self.S.op("dve", lambda e: e.scalar_tensor_tensor(out=o, in0=a, scalar=scalar, in1=b, op0=op0, op1=op1),
                  reads=[at, bt] + list(extra_reads), writes=[ot])

    def cp(self, eng, ot, o, it, i):
        if eng == "act":
            self.act(ot, o, it, i, AF.Copy)
        else:
            self.S.op(eng, lambda e: e.tensor_copy(out=o, in_=i), reads=[it], writes=[ot])

    def memset(self, eng, ot, o, val):
        self.S.op(eng, lambda e: e.memset(o, val), reads=[], writes=[ot])

    def build(self):
        nc, S = self.nc, self.S
        din = self.din
        xq = din("xq", [2056, D]); xkv = din("xkv", [SEQ, D]); cT = din("cT", [128, 16])
        w_ada = din("w_ada", [D, 6 * D]); b_adaT = din("b_adaT", [128, 96])
        gmixT = din("g_mixT", [128, 16]); gffnT = din("g_ffnT", [128, 16]); gfinT = din("g_finT", [128, 16])
        w_in = din("w_in", [D, PIN]); w_o_sb = din("w_o_sb", [1024, D]); w_o_dsa = din("w_o_dsa", [1024, D])
        w_out = din("w_out", [D, D]); rel_bias = din("rel_bias", [32, 8])
        w_gate = din("w_gate", [D, DFF]); w_up = din("w_up", [D, DFF])
        convT = din("convT", [128, NFF * 3]); convb = din("convb", [128, NFF]); w_down = din("w_down", [DFF, D])
        identd = din("ident", [128, 128]); trid = din("tri", [128, 128])
        wcd = din("wc", [128, WCW]); wcapd = din("wcap", [128, WCW]); hcapd = din("hcap", [8, SEQ])
        hvd = din("hv", [128, 8]); ohbd = din("ohb", [32, LGV]); czd = din("cz", [8, LGV])
        yout = self.nc.dram_tensor("y", [2048, D], F32, kind="ExternalOutput")
        yout = Tk(yout.ap(), "y"); self.dr["y"] = yout
        KT_sb = self.dscr("KT_sb", [8, 128, SEQ], BF16); V_sb = self.dscr("V_sb", [NKB, 128, 1024], BF16)
        KT_ds = self.dscr("KT_ds", [8, 128, SEQ], BF16); V_ds = self.dscr("V_ds", [NKB, 128, 1024], BF16)
        KI = self.dscr("KI", [64, SEQ], BF16); gv = self.dscr("gv", [8, LGV], BF16)
        mTd = self.dscr("mTd", [NKB, 128, TW], BF16)
        self.P = Scope(self, "P")
        P = self.P
        with P:
            self._build_body(locals())
        return nc

    def _build_body(self, L):
        nc, S, P = self.nc, self.S, self.P
        g = L
        ident = P.sb("ident", [128, 128], F32); identb = P.sb("identb", [128, 128], BF16)
        trib = P.sb("trib", [128, 128], BF16); onesb = P.sb("onesb", [128, 128], BF16)
        onesf = P.sb("onesf", [128, 128], F32)
        wc = P.sb("wc", [128, WCW], BF16)
        hv = P.sb("hv", [128, 8], F32)
        modT = P.sb("modT", [128, 96], F32); gm1 = P.sb("gm1", [128, 16], F32); gm2 = P.sb("gm2", [128, 16], F32)
        gfin = P.sb("gfin", [128, 16], F32); cw = P.sb("cw", [128, NFF * 3], F32); cb = P.sb("cb", [128, NFF], F32)
        epsc = P.sb("epsc", [128, 1], F32)
        FHc = P.sb("FHc", [128, NKB, 8], BF16); FHd = P.sb("FHd", [128, 8, NKB * 8], BF16)
        a_halo = P.sb("a_halo", [128, NFF, 8], F32)
        self.c = dict(ident=ident, identb=identb, trib=trib, onesb=onesb, onesf=onesf, wc=wc,
                      hv=hv, modT=modT, gm1=gm1, gm2=gm2, gfin=gfin, cw=cw, cb=cb, epsc=epsc, FHc=FHc, FHd=FHd,
                      a_halo=a_halo)
        self.g = g
        try:
            self.setup()
            self.check_stop("setup")
            self.kv_phase()
            self.check_stop("kv")
            self.query_tile(None)
            self.check_stop("halo")
            for r in range(NTILE):
                self.query_tile(r)
                self.check_stop("tile%d" % r)
        except StopBuild:
            pass
        self.finish_debug()

    def check_stop(self, name):
        if self.stop_after == name:
            raise StopBuild()

    def finish_debug(self):
        self.S.barrier()

    def setup(self):
        S, c, g = self.S, self.c, self.g
        with Scope(self, "su") as sc:
            st32 = Rot([sc.sb("st32_%d" % i, [128, 2048], F32) for i in range(3)])
            S.dma("sp", c["ident"][:], g["identd"][:, :], reads=[g["identd"]], writes=[c["ident"]])
            S.dma("sp", c["hv"][:], g["hvd"][:, :], reads=[g["hvd"]], writes=[c["hv"]])
            S.dma("sp", c["gfin"][:], g["gfinT"][:, :], reads=[g["gfinT"]], writes=[c["gfin"]])
            S.dma("sp", c["cw"][:], g["convT"][:, :], reads=[g["convT"]], writes=[c["cw"]])
            S.dma("sp", c["cb"][:], g["convb"][:, :], reads=[g["convb"]], writes=[c["cb"]])
            self.memset("dve", c["onesb"], c["onesb"][:], 1.0)
            self.memset("dve", c["onesf"], c["onesf"][:], 1.0)
            self.memset("dve", c["epsc"], c["epsc"][:], 1e-6)
            t = st32.next()
            S.dma("sp", t[:, 0:128], g["identd"][:, :], reads=[g["identd"]], writes=[t])
            self.cp("dve", c["identb"], c["identb"][:], t, t[:, 0:128])
            t = st32.next()
            S.dma("sp", t[:, 0:128], g["trid"][:, :], reads=[g["trid"]], writes=[t])
            self.cp("dve", c["trib"], c["trib"][:], t, t[:, 0:128])
            for c0 in range(0, WCW, 2048):
                n = min(2048, WCW - c0)
                t = st32.next()
                S.dma("sp", t[:, 0:n], g["wcd"][:, c0:c0 + n], reads=[g["wcd"]], writes=[t])
                self.cp("dve", c["wc"], c["wc"][:, c0:c0 + n], t, t[:, 0:n])
            cact = sc.sb("cact", [128, 16], F32); csig = sc.sb("csig", [128, 16], F32)
            badd = sc.sb("badd", [128, 96], F32); gmx = sc.sb("gmx", [128, 16], F32); gff = sc.sb("gff", [128, 16], F32)
            S.dma("sp", cact[:], g["cT"][:, :], reads=[g["cT"]], writes=[cact])
            S.dma("sp", badd[:], g["b_adaT"][:, :], reads=[g["b_adaT"]], writes=[badd])
            S.dma("sp", gmx[:], g["gmixT"][:, :], reads=[g["gmixT"]], writes=[gmx])
            S.dma("sp", gff[:], g["gffnT"][:, :], reads=[g["gffnT"]], writes=[gff])
            self.act(csig, csig[:], cact, cact[:], AF.Sigmoid)
            self.tt("dve", cact, cact[:], cact, cact[:], csig, csig[:], ALU.mult)
            modp = sc.ps("modp", [128, 96], F32)
            for n in range(96):
                t = st32.next()
                S.dma("sp", t[:, :].rearrange("p (k n) -> p k n", k=16),
                      g["w_ada"][:, 128 * n:128 * n + 128].rearrange("(k p) n -> p k n", p=128),
                      reads=[g["w_ada"]], writes=[t])
                for k in range(16):
                    self.mm(modp, modp[:, n:n + 1], t, t[:, 128 * k:128 * k + 128], cact, cact[:, k:k + 1],
                            start=(k == 0), stop=(k == 15))
            self.tt("dve", c["modT"], c["modT"][:], modp, modp[:], badd, badd[:], ALU.add)
            m = c["modT"]
            self.stt(c["gm1"], c["gm1"][:], m, m[:, 16:32], 1.0, gmx, gmx[:], ALU.add, ALU.mult)
            self.stt(c["gm2"], c["gm2"][:], m, m[:, 64:80], 1.0, gff, gff[:], ALU.add, ALU.mult)
            rb = sc.sb("rb", [32, 8], F32); nb31 = sc.sb("nb31", [8, 1], F32)
            ohb = sc.sb("ohb", [32, LGV], F32); cz = sc.sb("cz", [8, LGV], F32)
            gvs = sc.sb("gvs", [8, LGV], F32); gvb = sc.sb("gvb", [8, LGV], BF16)
            S.dma("sp", rb[:], g["rel_bias"][:, :], reads=[g["rel_bias"]], writes=[rb])
            S.dma("sp", nb31[:], g["rel_bias"][31:32, :].rearrange("a h -> h a"), reads=[g["rel_bias"]], writes=[nb31])
            S.dma("sp", ohb[:], g["ohbd"][:, :], reads=[g["ohbd"]], writes=[ohb])
            S.dma("sp", cz[:], g["czd"][:, :], reads=[g["czd"]], writes=[cz])
            self.ts("dve", nb31, nb31[:], nb31, nb31[:], -1.0, ALU.mult)
            gps = sc.ps("gps", [8, 512], F32)
            for c0 in range(0, LGV, 512):
                n = min(512, LGV - c0)
                self.mm(gps, gps[:, 0:n], rb, rb[:], ohb, ohb[:, c0:c0 + n], start=True, stop=True)
                self.act(gvs, gvs[:, c0:c0 + n], gps, gps[:, 0:n], AF.Exp, bias=nb31[:, 0:1], extra_reads=[nb31])
            self.tt("dve", gvb, gvb[:], gvs, gvs[:], cz, cz[:], ALU.mult)
            S.dma("sp", g["gv"][:, :], gvb[:], reads=[gvb], writes=[g["gv"]])
            wd_all = sc.sb("wd_all", [128, 8, WCW], BF16)
            for h in range(8):
                S.dma("sp", wd_all[:, h, :], bass.AP(g["gv"].t.tensor, h * LGV, [[1, 128], [1, WCW]]),
                      reads=[g["gv"]], writes=[wd_all], partial=True)
            FHc, FHd = c["FHc"], c["FHd"]
            for r in range(4):
                lo = max(0, 48 - 16 * r); hi = min(63, 68 - 16 * r)
                nn = hi - lo + 1
                c0 = 128 * (lo + 16 * r - 48)
                targets = [(FHc, lambda a, b: FHc[:, a:b, 2 * r:2 * r + 2], c["wc"], c["wc"][:, 0:WCW])]
                for h in range(8):
                    targets.append((FHd,
                                    (lambda a, b, h=h: FHd[:, h, :].rearrange("p (k e) -> p k e", e=8)[:, a:b, 2 * r:2 * r + 2]),
                                    wd_all, wd_all[:, h, :]))
                for (dst, dfn, srct, srcap) in targets:
                    if lo > 0:
                        self.memset("dve", dst, dfn(0, lo), 0.0)
                    if hi < 63:
                        self.memset("dve", dst, dfn(hi + 1, 64), 1.0)
                    self.cp("dve", dst, dfn(lo, hi + 1), srct, sub_ap(srcap, c0, [[128, nn], [1, 2]]))
            if self.debug:
                dbg = self.dscr("dbg_mod", [128, 96], F32)
                S.dma("sp", dbg[:, :], c["modT"][:], reads=[c["modT"]], writes=[dbg])
                dbg2 = self.dscr("dbg_fhd", [128, 8 * NKB * 8], BF16)
                S.dma("sp", dbg2[:, :], FHd[:].rearrange("p h k -> p (h k)"), reads=[FHd], writes=[dbg2])
                dbg3 = self.dscr("dbg_fhc", [128, NKB * 8], BF16)
                S.dma("sp", dbg3[:, :], FHc[:].rearrange("p k e -> p (k e)"), reads=[FHc], writes=[dbg3])

    def wload(self, src, rows_ap_fn, nk, ncols, dst, dst_ap):
        per = max(1, 2048 // ncols)
        for k0 in range(0, nk, per):
            k1 = min(nk, k0 + per)
            t = self.wst.next()
            n = (k1 - k0) * ncols
            self.S.dma("sp", t[:, 0:n].rearrange("p (k n) -> p k n", n=ncols), rows_ap_fn(k0, k1),
                       reads=[src], writes=[t])
            self.cp(self.cast_eng(), dst, dst_ap(k0, k1), t, t[:, 0:n].rearrange("p (k n) -> p k n", n=ncols))

    def cast_eng(self):
        self._ce = getattr(self, "_ce", 0) + 1
        return "act"

    def wload_cols(self, wt, c0, ncols, dst, nk=16, dcol0=0):
        self.wload(wt, lambda k0, k1: wt[128 * k0:128 * k1, c0:c0 + ncols].rearrange("(k p) n -> p k n", p=128),
                   nk, ncols, dst, lambda k0, k1: dst[:, k0:k1, dcol0:dcol0 + ncols])

    def norm_tile(self, sc, xsrc, row0, subs, xT, hT, hcol0, gm, sh_col0, do_norm=True):
        S, c = self.S, self.c
        Nt = sum(n for _, n in subs)
        col = 0
        for (ro, nr) in subs:
            xr = self.xrow.next()
            S.dma("sp", xr[0:nr, :], xsrc[row0 + ro:row0 + ro + nr, :], reads=[xsrc], writes=[xr])
            for gq in range(4):
                pt = self.ptr.next()
                for i in range(4):
                    fc = 4 * gq + i
                    self.tr(pt, pt[:, i * nr:(i + 1) * nr], xr, xr[0:nr, 128 * fc:128 * fc + 128],
                            c["ident"], c["ident"][0:nr, 0:nr])
                self.cp("act", xT, xT[:, 4 * gq:4 * gq + 4, col:col + nr],
                        pt, pt[:, 0:4 * nr].rearrange("p (i n) -> p i n", n=nr))
            col += nr
        if do_norm:
            self.norm_from_xT(sc, xT, Nt, hT, hcol0, gm, sh_col0)

    def norm_stats(self, xT, Nt):
        c = self.c
        ss = self.pss.next()
        for fc in range(FC):
            sq = self.sqb.next()
            self.act(sq, sq[:, 0:Nt], xT, xT[:, fc, 0:Nt], AF.Square)
            self.mm(ss, ss[:, 0:Nt], c["onesf"], c["onesf"][:], sq, sq[:, 0:Nt], start=(fc == 0), stop=(fc == FC - 1))
        rs = self.rstd
        self.act(rs, rs[:, 0:Nt], ss, ss[:, 0:Nt], AF.Sqrt, scale=1.0 / D, bias=c["epsc"][:, 0:1],
                 extra_reads=[c["epsc"]])
        self.S.op("dve", lambda e: e.reciprocal(out=rs[:, 0:Nt], in_=rs[:, 0:Nt]), reads=[rs], writes=[rs])
        return rs

    def norm_from_xT(self, sc, xT, Nt, hT, hcol0, gm, sh_col0):
        c = self.c
        rs = self.norm_stats(xT, Nt)
        for fc in range(FC):
            tmp = self.sqb.next()
            self.tt("dve", tmp, tmp[:, 0:Nt], xT, xT[:, fc, 0:Nt], rs, rs[:, 0:Nt], ALU.mult)
            self.act(hT, hT[:, fc, hcol0:hcol0 + Nt], tmp, tmp[:, 0:Nt], AF.Identity, scale=gm[:, fc:fc + 1],
                     bias=c["modT"][:, sh_col0 + fc:sh_col0 + fc + 1], extra_reads=[gm, c["modT"]])

    def kv_phase(self):
        S, c, g = self.S, self.c, self.g
        NSUP = 1024
        with Scope(self, "kv") as sc:
            self.wst = Rot([sc.sb("wst%d" % i, [128, 2048], F32) for i in range(3)])
            self.xrow = Rot([sc.sb("xrow%d" % i, [128, D], F32) for i in range(2)])
            self.ptr = Rot([sc.ps("ptr%d" % i, [128, 512], F32) for i in range(2)])
            self.pss = Rot([sc.ps("pss", [128, 512], F32)])
            self.sqb = Rot([sc.sb("sqb%d" % i, [128, 512], F32) for i in range(2)])
            self.rstd = sc.sb("rstd", [128, 512], F32)
            xT = sc.sb("xT", [128, FC, 512], F32)
            hTb = sc.sb("hTb", [128, FC, NSUP], BF16)
            w128 = Rot([sc.sb("w128_%d" % i, [128, FC, 128], BF16) for i in range(2)])
            w512 = Rot([sc.sb("w512_%d" % i, [128, FC, 512], BF16) for i in range(2)])
            stgK = Rot([sc.sb("stgK%d" % i, [128, NSUP], BF16) for i in range(2)])
            stgV = Rot([sc.sb("stgV%d" % i, [128, NSUP // 128, 512], BF16) for i in range(2)])
            pmm = Rot([sc.ps("pmm%d" % i, [128, 512], F32) for i in range(4)])
            nsup = SEQ // NSUP
            import os as _os
            kvstage = int(_os.environ.get("KVSTAGE", "9"))
            if kvstage < 9:
                nsup = 1
            ev = 0
            for T in range(nsup):
                for q in range(NSUP // 512):
                    self.norm_tile(sc, g["xkv"], NSUP * T + 512 * q, [(128 * i, 128) for i in range(4)], xT, hTb, 512 * q,
                                   c["gm1"], 0)
                if kvstage < 2:
                    self.dump("dbg_hTb", hTb, hTb[:].rearrange("p a b -> p (a b)"), [128, FC * NSUP], BF16)
                    continue
                kch = [(C_KSB + 128 * h, 128, g["KT_sb"], h) for h in range(8)]
                kch += [(C_KDS + 128 * h, 128, g["KT_ds"], h) for h in range(8)]
                kch += [(C_KIX, 64, g["KI"], None)]
                for (c0, ncols, dst, h) in kch:
                    wt = w128.next()
                    self.wload_cols(g["w_in"], c0, ncols, wt)
                    stg = stgK.next()
                    for q in range(NSUP // 512):
                        ps = pmm.next()
                        for fc in range(FC):
                            self.mm(ps, ps[0:ncols, :], wt, wt[:, fc, 0:ncols], hTb, hTb[:, fc, 512 * q:512 * q + 512],
                                    start=(fc == 0), stop=(fc == FC - 1))
                        ev += 1
                        self.cp("act" if ev % 2 else "dve", stg, stg[0:ncols, 512 * q:512 * q + 512], ps, ps[0:ncols, :])
                    if h is None:
                        S.dma("act", dst[:, NSUP * T:NSUP * T + NSUP], stg[0:64, :], reads=[stg], writes=[dst], partial=True)
                    else:
                        S.dma("act", dst[h, :, NSUP * T:NSUP * T + NSUP], stg[:, :], reads=[stg], writes=[dst], partial=True)
                if kvstage < 3:
                    continue
                for (c0, dst) in ((C_VSB, g["V_sb"]), (C_VDS, g["V_ds"])):
                    for half in range(2):
                        wt = w512.next()
                        for qq in range(4):
                            self.wload_cols(g["w_in"], c0 + 512 * half + 128 * qq, 128, wt, dcol0=128 * qq)
                        stg = stgV.next()
                        for tb in range(NSUP // 128):
                            ps = pmm.next()
                            for fc in range(FC):
                                self.mm(ps, ps[:, :], hTb, hTb[:, fc, 128 * tb:128 * tb + 128], wt, wt[:, fc, :],
                                        start=(fc == 0), stop=(fc == FC - 1))
                            ev += 1
                            self.cp("act" if ev % 2 else "dve", stg, stg[:, tb, :], ps, ps[:, :])
                        kb0 = (NSUP // 128) * T
                        S.dma("act", dst[kb0:kb0 + NSUP // 128, :, 512 * half:512 * half + 512].rearrange("k p n -> p k n"),
                              stg[:, :, :], reads=[stg], writes=[dst], partial=True)

    def query_tile(self, r):
        S, c, g = self.S, self.c, self.g
        halo = r is None
        Nq = 8 if halo else TW
        subs = [(0, 8)] if halo else [(128 * i, 128) for i in range(4)]
        row0 = 2048 if halo else TW * r
        KB = NKB if halo else 16 * (r + 1)
        if self.debug == "short":
            KB = min(KB, 32)
        tname = "h" if halo else "t%d" % r
        with Scope(self, tname) as ts:
            hT = ts.sb("hT", [128, FC, Nq], BF16)
            oTsb = ts.sb("oTsb", [128, 8, Nq], BF16); oTds = ts.sb("oTds", [128, 8, Nq], BF16)
            with Scope(self, tname + "Q") as tq:
                QsbT = tq.sb("QsbT", [128, 8, Nq], BF16); QdsT = tq.sb("QdsT", [128, 8, Nq], BF16)
                QiT = tq.sb("QiT", [128, 8, Nq], BF16); wis = tq.sb("wis", [128, len(subs), 16], F32)
                self.memset("dve", wis, wis[:], 0.0)
                with Scope(self, tname + "A") as sc:
                    self.wst = Rot([sc.sb("wst%d" % i, [128, 2048], F32) for i in range(3)])
                    self.xrow = Rot([sc.sb("xrow%d" % i, [128, D], F32) for i in range(2)])
                    self.ptr = Rot([sc.ps("ptr%d" % i, [128, 512], F32) for i in range(2)])
                    self.pss = Rot([sc.ps("pss", [128, 512], F32)])
                    self.sqb = Rot([sc.sb("sqb%d" % i, [128, 512], F32) for i in range(2)])
                    self.rstd = sc.sb("rstd", [128, 512], F32)
                    xT = sc.sb("xT", [128, FC, Nq], F32)
                    w128 = Rot([sc.sb("w128_%d" % i, [128, FC, 128], BF16) for i in range(3)])
                    pmm = Rot([sc.ps("pmm%d" % i, [128, 512], F32) for i in range(3)])
                    self.norm_tile(sc, g["xq"], row0, subs, xT, hT, 0, c["gm1"], 0)
                    ev = 0
                    for (cbase, dstT) in ((C_QSB, QsbT), (C_QDS, QdsT), (C_QIX, QiT)):
                        for h in range(8):
                            wt = w128.next()
                            self.wload_cols(g["w_in"], cbase + 128 * h, 128, wt)
                            ps = pmm.next()
                            for fc in range(FC):
                                self.mm(ps, ps[:, 0:Nq], wt, wt[:, fc, :], hT, hT[:, fc, :], start=(fc == 0), stop=(fc == FC - 1))
                            ev += 1
                            self.cp("act" if ev % 2 else "dve", dstT, dstT[:, h, :], ps, ps[:, 0:Nq])
                    wt = w128.next()
                    self.wload_cols(g["w_in"], C_WIX, 16, wt)
                    col = 0
                    for si, (ro, nr) in enumerate(subs):
                        ps = pmm.next()
                        for fc in range(FC):
                            self.mm(ps, ps[0:nr, 0:16], hT, hT[:, fc, col:col + nr], wt, wt[:, fc, 0:16],
                                    start=(fc == 0), stop=(fc == FC - 1))
                        self.act(wis, wis[0:nr, si, :], ps, ps[0:nr, 0:16], AF.Copy, scale=WIS_SCALE)
                        col += nr
                if self.debug:
                    self.dump("dbg_hT_" + tname, hT, hT[:].rearrange("p a b -> p (a b)"), [128, FC * Nq], BF16)
                    self.dump("dbg_QiT_" + tname, QiT, QiT[:].rearrange("p a b -> p (a b)"), [128, 8 * Nq], BF16)
                    self.dump("dbg_wis_" + tname, wis, wis[:].rearrange("p a b -> p (a b)"), [128, len(subs) * 16], F32)
                self.check_stop(tname + "A")
                self.sb_attention(tname, r, Nq, KB, QsbT, oTsb)
                if self.debug:
                    self.dump("dbg_oTsb_" + tname, oTsb, oTsb[:].rearrange("p a b -> p (a b)"), [128, 8 * Nq], BF16)
                self.check_stop(tname + "B")
                self.indexer(tname, r, Nq, subs, KB, QiT, wis)
                self.check_stop(tname + "C")
                self.dsa_attention(tname, r, Nq, KB, QdsT, oTds)
                if self.debug:
                    self.dump("dbg_oTds_" + tname, oTds, oTds[:].rearrange("p a b -> p (a b)"), [128, 8 * Nq], BF16)
                self.check_stop(tname + "D")
            self.post_attention(tname, r, Nq, subs, row0, hT, oTsb, oTds)

    def dump(self, name, t, ap, shape, dt):
        d = self.dscr(name, shape, dt)
        self.S.dma("sp", d[:, :], ap, reads=[t], writes=[d])

    def kv_chunk_load(self, KTd, Vd, h, cidx, KTc, Vc):
        S = self.S
        S.dma("sp", KTc[:, :], KTd[h, :, 2048 * cidx:2048 * cidx + 2048], reads=[KTd], writes=[KTc])
        S.dma("sp", Vc[:, :, :], Vd[16 * cidx:16 * cidx + 16, :, 128 * h:128 * h + 128].rearrange("k p n -> p k n"),
              reads=[Vd], writes=[Vc])

    def sb_attention(self, tname, r, Nq, KB, QsbT, oTsb):
        S, c, g = self.S, self.c, self.g
        halo = r is None
        with Scope(self, tname + "B") as sc:
            KTc = Rot([sc.sb("KTc%d" % i, [128, 2048], BF16) for i in range(3)])
            Vc = Rot([sc.sb("Vc%d" % i, [128, 16, 128], BF16) for i in range(3)])
            e32 = Rot([sc.sb("e32_%d" % i, [128, Nq], F32) for i in range(7)])
            spb = Rot([sc.sb("spb%d" % i, [128, Nq], BF16) for i in range(5)])
            w32 = Rot([sc.sb("w32_%d" % i, [128, Nq], F32) for i in range(3)])
            ab = Rot([sc.sb("ab%d" % i, [128, Nq], BF16) for i in range(3)])
            Rbf = Rot([sc.sb("Rbf%d" % i, [128, Nq], BF16) for i in range(4)])
            LT = Rot([sc.ps("LT%d" % i, [128, 512], F32) for i in range(3)])
            CP = Rot([sc.ps("CP%d" % i, [128, 512], F32) for i in range(3)])
            ACC = Rot([sc.ps("ACC%d" % i, [128, 512], F32) for i in range(2)])
            steps = []
            for h in range(8):
                for kb in range(KB - 1, -1, -1):
                    steps.append(dict(h=h, kb=kb, first=(kb == KB - 1), last=(kb == 0)))
            st = dict(ktc=None, vc=None, acc=None, rb=None)

            def g0(d):
                kb, h = d["kb"], d["h"]
                kbl = kb % 16
                if kbl == 15 or d["first"]:
                    st["ktc"] = KTc.next(); st["vc"] = Vc.next()
                    self.kv_chunk_load(g["KT_sb"], g["V_sb"], h, kb // 16, st["ktc"], st["vc"])
                if d["first"]:
                    st["acc"] = ACC.next()
                d["vc"] = st["vc"]; d["acc"] = st["acc"]
                ktc = st["ktc"]
                lt = LT.next()
                d["lt"] = lt
                self.mm(lt, lt[:, 0:Nq], ktc, ktc[:, 128 * kbl:128 * kbl + 128], QsbT, QsbT[:, h, :], True, True)

            def g1(d):
                kb = d["kb"]
                lt = d["lt"]
                e = e32.next()
                d["e"] = e
                self.act(e, e[:, :], lt, lt[:, 0:Nq], AF.Exp, scale=SCALE)
                if halo:
                    self.tt("dve", e, e[:, :], e, e[:, :], c["FHc"], c["FHc"][:, 63 - kb, :], ALU.mult)
                else:
                    kk = kb - 16 * r + 1
                    if 0 <= kk <= 16:
                        o0 = 2 + 128 * (16 - kk)
                        self.tt("dve", e, e[:, :], e, e[:, :], c["wc"], c["wc"][:, o0:o0 + Nq], ALU.mult)

            def g2(d):
                e = d["e"]
                sp = spb.next()
                d["sp"] = sp
                self.act(sp, sp[:, :], e, e[:, :], AF.Ln, bias=1.0)

            def g3(d):
                sp = d["sp"]
                cp_ = CP.next()
                d["cp"] = cp_
                first = d["first"]
                self.mm(cp_, cp_[:, 0:Nq], c["trib"], c["trib"][:, :], sp, sp[:, :], True, first)
                if not first:
                    rb = st["rb"]
                    self.mm(cp_, cp_[:, 0:Nq], c["onesb"], c["onesb"][:, :], rb, rb[:, :], False, True)
                if not d["last"]:
                    if first:
                        st["rb"] = sp
                    else:
                        rn = Rbf.next()
                        self.tt("dve", rn, rn[:, :], st["rb"], st["rb"][:, :], sp, sp[:, :], ALU.add)
                        st["rb"] = rn

            def g4(d):
                cp_ = d["cp"]
                w = w32.next()
                d["w"] = w
                self.act(w, w[:, :], cp_, cp_[:, 0:Nq], AF.Exp, scale=-1.0)

            def g5(d):
                e, w = d["e"], d["w"]
                a = ab.next()
                d["a"] = a
                self.tt("dve", a, a[:, :], e, e[:, :], w, w[:, :], ALU.mult)

            def g6(d):
                a, acc, vc = d["a"], d["acc"], d["vc"]
                self.mm(acc, acc[:, 0:Nq], vc, vc[:, d["kb"] % 16, :], a, a[:, :], d["first"], d["last"])
                if d["last"]:
                    self.cp("act", oTsb, oTsb[:, d["h"], :], acc, acc[:, 0:Nq])

            self.pipeline(steps, [g0, g1, g2, g3, g4, g5, g6])

    def pipeline(self, steps, stages):
        n = len(steps)
        ns = len(stages)
        for i in range(n + ns - 1):
            for k, fn in enumerate(stages):
                j = i - k
                if 0 <= j < n:
                    fn(steps[j])

    def indexer(self, tname, r, Nq, subs, KB, QiT, wis):
        S, c, g = self.S, self.c, self.g
        halo = r is None
        Lk = 128 * KB
        with Scope(self, tname + "C") as sc:
            KI2 = sc.sb("KI2", [128, Lk], BF16)
            scores = Rot([sc.sb("score%d" % i, [128, Lk], F32) for i in range(2)])
            maskb = sc.sb("maskb", [128, Lk], BF16)
            m8 = sc.sb("m8", [128, 8], F32)
            rl = Rot([sc.sb("rl%d" % i, [128, 512], BF16) for i in range(4)])
            Dg = Rot([sc.sb("Dg%d" % i, [128, 16, 128], BF16) for i in range(2)])
            mstg = Rot([sc.sb("mstg%d" % i, [128, 8, 128], BF16) for i in range(2)])
            SC = Rot([sc.ps("SC%d" % i, [128, 512], F32) for i in range(4)])
            SA = Rot([sc.ps("SA%d" % i, [128, 512], F32) for i in range(2)])
            TP = Rot([sc.ps("TP%d" % i, [128, 1024], BF16) for i in range(2)])
            if halo:
                cap = sc.sb("hcap", [8, Lk], F32)
                S.dma("sp", cap[:, :], g["hcapd"][:, 0:Lk], reads=[g["hcapd"]], writes=[cap])
            else:
                cap = sc.sb("wcap", [128, WCW], F32)
                S.dma("sp", cap[:, :], g["wcapd"][:, :], reads=[g["wcapd"]], writes=[cap])
            S.dma("sp", KI2[0:64, :], g["KI"][:, 0:Lk], reads=[g["KI"]], writes=[KI2], partial=True)
            S.dma("sp", KI2[64:128, :], g["KI"][:, 0:Lk], reads=[g["KI"]], writes=[KI2], partial=True)
            cols = []
            col = 0
            for (ro, nr) in subs:
                cols.append(col)
                col += nr
            sstate = {}

            def idx_phase(si):
                ro, nr = subs[si]
                col = cols[si]
                score = scores.next()
                sstate[si] = score
                dg = Dg.next()
                for hi in range(16):
                    self.ts("dve", dg, dg[0:nr, hi, 0:nr], c["identb"], c["identb"][0:nr, 0:nr], wis[0:nr, si, hi:hi + 1],
                            ALU.mult, extra_reads=[wis])
                steps = [dict(ch=ch, hi=hi) for ch in range(Lk // 512) for hi in range(16)]
                cur = {}

                def s1(d):
                    hi, ch = d["hi"], d["ch"]
                    b0 = 64 * (hi % 2)
                    ps = SC.next()
                    d["ps"] = ps
                    self.mm(ps, ps[0:nr, :], QiT, QiT[b0:b0 + 64, hi // 2, col:col + nr],
                            KI2, KI2[b0:b0 + 64, 512 * ch:512 * ch + 512], True, True)

                def s2(d):
                    t = rl.next()
                    d["t"] = t
                    self.act(t, t[0:nr, :], d["ps"], d["ps"][0:nr, :], AF.Relu)

                def s3(d):
                    hi, ch = d["hi"], d["ch"]
                    if hi == 0:
                        cur["sa"] = SA.next()
                    sa = cur["sa"]
                    t = d["t"]
                    self.mm(sa, sa[0:nr, :], dg, dg[0:nr, hi, 0:nr], t, t[0:nr, :], hi == 0, hi == 15)
                    if hi == 15:
                        self.cp("act", score, score[0:nr, 512 * ch:512 * ch + 512], sa, sa[0:nr, :])

                self.pipeline(steps, [s1, s2, s3])

            def topk_phase(si):
                ro, nr = subs[si]
                col = cols[si]
                score = sstate[si]
                if halo:
                    self.tt("dve", score, score[0:nr, :], score, score[0:nr, :], cap, cap[0:nr, 0:Lk], ALU.min)
                else:
                    for kk in range(17):
                        kb = 16 * r - 1 + kk
                        if kb < 0 or kb >= KB:
                            continue
                        o0 = 2 + 128 * (16 - kk + si)
                        self.tt("dve", score, score[0:nr, 128 * kb:128 * kb + 128], score, score[0:nr, 128 * kb:128 * kb + 128],
                                cap, cap[0:nr, o0:o0 + 128], ALU.min)
                for it in range(32):
                    S.op("dve", lambda e: e.max(out=m8[0:nr, :], in_=score[0:nr, :]), reads=[score], writes=[m8])
                    S.op("dve", lambda e: e.match_replace(out=score[0:nr, :], in_to_replace=m8[0:nr, :],
                                                          in_values=score[0:nr, :], imm_value=NEG),
                         reads=[score, m8], writes=[score])
                self.ts("dve", maskb, maskb[0:nr, :], score, score[0:nr, :], -2.0e38, ALU.is_le)
                for kb0 in range(0, KB, 8):
                    tp = TP.next()
                    for i in range(8):
                        kb = kb0 + i
                        self.tr(tp, tp[:, 128 * i:128 * i + nr], maskb, maskb[0:nr, 128 * kb:128 * kb + 128],
                                c["identb"], c["identb"][0:nr, 0:nr])
                    ms = mstg.next()
                    self.cp("act", ms, ms[:, :, 0:nr], tp, tp[:, :].rearrange("p (i n) -> p i n", n=128)[:, :, 0:nr])
                    S.dma("act", g["mTd"][kb0:kb0 + 8, :, col:col + nr].rearrange("k p n -> p k n"), ms[:, :, 0:nr],
                          reads=[ms], writes=[g["mTd"]], partial=True)

            nsub = len(subs)
            idx_phase(0)
            for si in range(nsub):
                if si + 1 < nsub:
                    idx_phase(si + 1)
                topk_phase(si)

    def dsa_attention(self, tname, r, Nq, KB, QdsT, oTds):
        S, c, g = self.S, self.c, self.g
        halo = r is None
        with Scope(self, tname + "D") as sc:
            KTc = Rot([sc.sb("KTc%d" % i, [128, 2048], BF16) for i in range(2)])
            Vc = Rot([sc.sb("Vc%d" % i, [128, 16, 128], BF16) for i in range(2)])
            mTc = Rot([sc.sb("mTc%d" % i, [128, 16, Nq], BF16) for i in range(2)])
            Wd = Rot([sc.sb("Wd%d" % i, [128, WCW], BF16) for i in range(2)])
            pb = Rot([sc.sb("pb%d" % i, [128, Nq], BF16) for i in range(4)])
            p2 = Rot([sc.sb("p2_%d" % i, [128, Nq], BF16) for i in range(4)])
            dens = sc.sb("dens", [128, Nq], F32)
            LT = Rot([sc.ps("LT%d" % i, [128, 512], F32) for i in range(3)])
            ACC = Rot([sc.ps("ACC%d" % i, [128, 512], F32) for i in range(2)])
            DEN = Rot([sc.ps("DEN%d" % i, [128, 512], F32) for i in range(2)])
            steps = []
            for h in range(8):
                for kb in range(KB):
                    steps.append(dict(h=h, kb=kb, first=(kb == 0), last=(kb == KB - 1)))
            st = dict(ktc=None, vc=None, mtc=None, acc=None, den=None, wd=None)

            def g0(d):
                h, kb = d["h"], d["kb"]
                kbl = kb % 16
                if d["first"]:
                    st["acc"] = ACC.next(); st["den"] = DEN.next()
                    if not halo:
                        wd = Wd.next()
                        st["wd"] = wd
                        S.dma("sp", wd[:, :], bass.AP(g["gv"].t.tensor, h * LGV, [[1, 128], [1, WCW]]),
                              reads=[g["gv"]], writes=[wd])
                if kbl == 0:
                    st["ktc"] = KTc.next(); st["vc"] = Vc.next(); st["mtc"] = mTc.next()
                    self.kv_chunk_load(g["KT_ds"], g["V_ds"], h, kb // 16, st["ktc"], st["vc"])
                    S.dma("sp", st["mtc"][:, :, :], g["mTd"][kb:kb + 16, :, 0:Nq].rearrange("k p n -> p k n"),
                          reads=[g["mTd"]], writes=[st["mtc"]])
                for k_ in ("vc", "mtc", "acc", "den", "wd"):
                    d[k_] = st[k_]
                ktc = st["ktc"]
                lt = LT.next()
                d["lt"] = lt
                self.mm(lt, lt[:, 0:Nq], ktc, ktc[:, 128 * kbl:128 * kbl + 128], QdsT, QdsT[:, h, :], True, True)

            def g1(d):
                lt = d["lt"]
                p = pb.next()
                d["p"] = p
                self.act(p, p[:, :], lt, lt[:, 0:Nq], AF.Exp, scale=SCALE)

            def g2(d):
                h, kb = d["h"], d["kb"]
                kbl = kb % 16
                p, mtc, wd = d["p"], d["mtc"], d["wd"]
                q2 = p2.next()
                d["q2"] = q2
                self.tt("dve", q2, q2[:, :], p, p[:, :], mtc, mtc[:, kbl, :], ALU.mult)
                if halo:
                    fh = c["FHd"][:, h, :].rearrange("p (k e) -> p k e", e=8)[:, 63 - kb, :]
                    self.tt("dve", q2, q2[:, :], q2, q2[:, :], c["FHd"], fh, ALU.mult)
                else:
                    kk = kb - 16 * r + 1
                    if 0 <= kk <= 16:
                        o0 = 2 + 128 * (16 - kk)
                        self.tt("dve", q2, q2[:, :], q2, q2[:, :], wd, wd[:, o0:o0 + Nq], ALU.mult)

            def g3(d):
                h, kb = d["h"], d["kb"]
                kbl = kb % 16
                q2, vc, acc, den = d["q2"], d["vc"], d["acc"], d["den"]
                self.mm(acc, acc[:, 0:Nq], vc, vc[:, kbl, :], q2, q2[:, :], d["first"], d["last"])
                self.mm(den, den[:, 0:Nq], c["onesb"], c["onesb"][:, :], q2, q2[:, :], d["first"], d["last"])
                if d["last"]:
                    self.ts("dve", dens, dens[:, :], den, den[:, 0:Nq], 1e-30, ALU.max)
                    S.op("dve", lambda e: e.reciprocal(out=dens[:, :], in_=dens[:, :]), reads=[dens], writes=[dens])
                    self.tt("dve", oTds, oTds[:, h, :], acc, acc[:, 0:Nq], dens, dens[:, :], ALU.mult)

            self.pipeline(steps, [g0, g1, g2, g3])

    def post_attention(self, tname, r, Nq, subs, row0, hT, oTsb, oTds):
        S, c, g = self.S, self.c, self.g
        halo = r is None
        m = c["modT"]
        with Scope(self, tname + "E") as se:
            xT = se.sb("xT", [128, FC, Nq], F32)
            self.wst = Rot([se.sb("wst%d" % i, [128, 2048], F32) for i in range(3)])
            self.sqb = Rot([se.sb("sqb%d" % i, [128, 512], F32) for i in range(2)])
            self.rstd = se.sb("rstd", [128, 512], F32)
            with Scope(self, tname + "E1") as sc:
                self.xrow = Rot([sc.sb("xrow%d" % i, [128, D], F32) for i in range(2)])
                self.ptr = Rot([sc.ps("ptr%d" % i, [128, 512], F32) for i in range(2)])
                merged = sc.sb("merged", [128, FC, Nq], BF16)
                wo = Rot([sc.sb("wo%d" % i, [128, 8, 128], BF16) for i in range(2)])
                w128 = Rot([sc.sb("w128_%d" % i, [128, FC, 128], BF16) for i in range(3)])
                sg = Rot([sc.sb("sg%d" % i, [128, Nq], F32) for i in range(2)])
                m1 = Rot([sc.sb("m1_%d" % i, [128, Nq], F32) for i in range(2)])
                m2 = Rot([sc.sb("m2_%d" % i, [128, Nq], F32) for i in range(2)])
                PA = Rot([sc.ps("PA%d" % i, [128, 512], F32) for i in range(2)])
                PG = Rot([sc.ps("PG%d" % i, [128, 512], F32) for i in range(2)])
                self.norm_tile(sc, g["xq"], row0, subs, xT, None, 0, None, 0, do_norm=False)
                for cc in range(FC):
                    parts = []
                    for (wo_d, oT, gcol, mm_) in ((g["w_o_sb"], oTsb, C_GSB, m1), (g["w_o_dsa"], oTds, C_GDS, m2)):
                        wt = wo.next()
                        self.wload_cols(wo_d, 128 * cc, 128, wt, nk=8)
                        pa = PA.next()
                        for h in range(8):
                            self.mm(pa, pa[:, 0:Nq], wt, wt[:, h, :], oT, oT[:, h, :], h == 0, h == 7)
                        wg = w128.next()
                        self.wload_cols(g["w_in"], gcol + 128 * cc, 128, wg)
                        pg = PG.next()
                        for fc in range(FC):
                            self.mm(pg, pg[:, 0:Nq], wg, wg[:, fc, :], hT, hT[:, fc, :], fc == 0, fc == FC - 1)
                        s_ = sg.next()
                        self.act(s_, s_[:, :], pg, pg[:, 0:Nq], AF.Sigmoid)
                        mt = mm_.next()
                        self.tt("dve", mt, mt[:, :], pa, pa[:, 0:Nq], s_, s_[:, :], ALU.mult)
                        parts.append(mt)
                    self.tt("pool", merged, merged[:, cc, :], parts[0], parts[0][:, :], parts[1], parts[1][:, :], ALU.add)
                for cc in range(FC):
                    wt = w128.next()
                    self.wload_cols(g["w_out"], 128 * cc, 128, wt)
                    pa = PA.next()
                    for k in range(FC):
                        self.mm(pa, pa[:, 0:Nq], wt, wt[:, k, :], merged, merged[:, k, :], k == 0, k == FC - 1)
                    self.stt(xT, xT[:, cc, :], pa, pa[:, 0:Nq], m[:, 32 + cc:33 + cc], xT, xT[:, cc, :], ALU.mult, ALU.add,
                             extra_reads=[m])
            if self.debug:
                self.dump("dbg_xpost_" + tname, xT, xT[:].rearrange("p a b -> p (a b)"), [128, FC * Nq], F32)
            with Scope(self, tname + "E2") as sc:
                self.pss = Rot([sc.ps("pss", [128, 512], F32)])
                PA = Rot([sc.ps("PA%d" % i, [128, 512], F32) for i in range(2)])
                PU = Rot([sc.ps("PU%d" % i, [128, 512], F32) for i in range(2)])
                self.norm_from_xT(sc, xT, Nq, hT, 0, c["gm2"], 48)
                uT = None if halo else sc.sb("uT", [128, NFF, Nq], BF16)
                with Scope(self, tname + "E2a") as sa:
                    w128 = Rot([sa.sb("w128_%d" % i, [128, FC, 128], BF16) for i in range(4)])
                    if not halo:
                        aext = Rot([sa.sb("aext%d" % i, [128, Nq + 2], F32) for i in range(2)])
                        yb = Rot([sa.sb("yb%d" % i, [128, Nq], F32) for i in range(2)])
                        sl = Rot([sa.sb("sl%d" % i, [128, Nq], F32) for i in range(2)])
                    for k in range(NFF):
                        wg = w128.next()
                        self.wload_cols(g["w_gate"], 128 * k, 128, wg)
                        pa = PA.next()
                        for fc in range(FC):
                            self.mm(pa, pa[:, 0:Nq], wg, wg[:, fc, :], hT, hT[:, fc, :], fc == 0, fc == FC - 1)
                        if halo:
                            self.tt("dve", c["a_halo"], c["a_halo"][:, k, :], pa, pa[:, 0:Nq], c["hv"], c["hv"][:, :], ALU.mult)
                            continue
                        wu = w128.next()
                        self.wload_cols(g["w_up"], 128 * k, 128, wu)
                        pu = PU.next()
                        for fc in range(FC):
                            self.mm(pu, pu[:, 0:Nq], wu, wu[:, fc, :], hT, hT[:, fc, :], fc == 0, fc == FC - 1)
                        ae = aext.next()
                        self.cp("act", ae, ae[:, 2:Nq + 2], pa, pa[:, 0:Nq])
                        self.cp("pool", ae, ae[:, 0:2], c["a_halo"], c["a_halo"][:, k, 2 * r:2 * r + 2])
                        y = yb.next()
                        cwk = c["cw"]
                        self.ts("dve", y, y[:, :], ae, ae[:, 0:Nq], cwk[:, 3 * k:3 * k + 1], ALU.mult,
                                c["cb"][:, k:k + 1], ALU.add, extra_reads=[cwk, c["cb"]])
                        self.stt(y, y[:, :], ae, ae[:, 1:Nq + 1], cwk[:, 3 * k + 1:3 * k + 2], y, y[:, :], ALU.mult, ALU.add,
                                 extra_reads=[cwk])
                        self.stt(y, y[:, :], ae, ae[:, 2:Nq + 2], cwk[:, 3 * k + 2:3 * k + 3], y, y[:, :], ALU.mult, ALU.add,
                                 extra_reads=[cwk])
                        s_ = sl.next()
                        self.act(s_, s_[:, :], y, y[:, :], AF.Silu)
                        self.tt("dve", uT, uT[:, k, :], pu, pu[:, 0:Nq], s_, s_[:, :], ALU.mult)
                if halo:
                    if self.debug:
                        self.dump("dbg_ahalo", c["a_halo"], c["a_halo"][:].rearrange("p a b -> p (a b)"), [128, NFF * 8], F32)
                    return
                with Scope(self, tname + "E2b") as sb_:
                    wdn = Rot([sb_.sb("wdn%d" % i, [128, NFF, 128], BF16) for i in range(2)])
                    for cc in range(FC):
                        wt = wdn.next()
                        self.wload_cols(g["w_down"], 128 * cc, 128, wt, nk=NFF)
                        pa = PA.next()
                        for k in range(NFF):
                            self.mm(pa, pa[:, 0:Nq], wt, wt[:, k, :], uT, uT[:, k, :], k == 0, k == NFF - 1)
                        self.stt(xT, xT[:, cc, :], pa, pa[:, 0:Nq], m[:, 80 + cc:81 + cc], xT, xT[:, cc, :], ALU.mult, ALU.add,
                                 extra_reads=[m])
                    rs = self.norm_stats(xT, Nq)
                    for cc in range(FC):
                        self.stt(xT, xT[:, cc, :], xT, xT[:, cc, :], c["gfin"][:, cc:cc + 1], rs, rs[:, 0:Nq], ALU.mult, ALU.mult,
                                 extra_reads=[c["gfin"]])
                    orow = Rot([sb_.sb("orow%d" % i, [128, D], F32) for i in range(2)])
                    PO = Rot([sb_.ps("PO%d" % i, [128, 512], F32) for i in range(2)])
                    ev = 0
                    for tb in range(4):
                        o = orow.next()
                        for gq in range(4):
                            po = PO.next()
                            for i in range(4):
                                cc = 4 * gq + i
                                self.tr(po, po[:, 128 * i:128 * i + 128], xT, xT[:, cc, 128 * tb:128 * tb + 128],
                                        c["ident"], c["ident"][:, :])
                            ev += 1
                            self.cp("act" if ev % 2 else "dve", o, o[:, 512 * gq:512 * gq + 512], po, po[:, :])
                        S.dma("sp", g["yout"][row0 + 128 * tb:row0 + 128 * tb + 128, :], o[:, :], reads=[o], writes=[g["yout"]],
                              partial=True)


def _bucket(delta):
    n = np.maximum(delta, 0)
    nf = np.maximum(n, 1).astype(np.float32)
    large = 16 + (np.log(nf / np.float32(16)) / np.float32(math.log(8.0)) * np.float32(16)).astype(np.int32)
    large = np.minimum(large, 31)
    return np.where(n < 16, n, large)


def _colT(v, k):
    return np.ascontiguousarray(np.asarray(v, np.float32).reshape(k, 128).T)


def make_core_inputs(inp, b, j):
    x = np.asarray(inp["x"], np.float32)
    xb = x[b]
    xq = np.zeros((2056, D), np.float32)
    for r in range(NTILE):
        t0 = TW * (4 * r + j)
        xq[TW * r:TW * r + TW] = xb[t0:t0 + TW]
        for e in range(2):
            t = t0 - 2 + e
            if t >= 0:
                xq[2048 + 2 * r + e] = xb[t]
    xkv = np.ascontiguousarray(xb.reshape(NKB, 128, D)[:, ::-1, :].reshape(SEQ, D))
    p = np.arange(128)[:, None]
    u = np.arange(WCW)[None, :]
    delta = (u - 2) + p + 512 * j - 2047
    wc = (delta >= 1).astype(np.float32)
    wcap = np.where(delta >= 0, BIG, -BIG).astype(np.float32)
    mm_ = np.arange(LGV)
    dm = mm_ - 2 + 512 * j - 2047
    ohb = np.zeros((32, LGV), np.float32)
    bk = _bucket(dm)
    valid = dm >= 0
    ohb[bk[valid], mm_[valid]] = 1.0
    cz = np.broadcast_to(valid.astype(np.float32)[None, :], (8, LGV)).copy()
    hcap = np.zeros((8, SEQ), np.float32)
    hv = np.ones((128, 8), np.float32)
    kbs = np.arange(NKB)[:, None]
    pp = np.arange(128)[None, :]
    for r in range(NTILE):
        for e in range(2):
            tpos = TW * (4 * r + j) - 2 + e
            spos = 128 * kbs + 127 - pp
            hcap[2 * r + e] = np.where(tpos - spos >= 0, BIG, -BIG).reshape(-1)
            if tpos < 0:
                hv[:, 2 * r + e] = 0.0
    d = {
        "xq": xq, "xkv": xkv, "cT": _colT(inp["c"][b], 16),
        "w_ada": np.ascontiguousarray(np.asarray(inp["w_ada"], np.float32)[0]),
        "b_adaT": _colT(np.asarray(inp["b_ada"])[0], 96),
        "g_mixT": _colT(np.asarray(inp["g_mix"])[0], 16), "g_ffnT": _colT(np.asarray(inp["g_ffn"])[0], 16),
        "g_finT": _colT(np.asarray(inp["g_final"]), 16),
        "w_in": np.ascontiguousarray(np.asarray(inp["w_in"], np.float32)[0]),
        "w_o_sb": np.ascontiguousarray(np.asarray(inp["w_o_sb"], np.float32)[0]),
        "w_o_dsa": np.ascontiguousarray(np.asarray(inp["w_o_dsa"], np.float32)[0]),
        "w_out": np.ascontiguousarray(np.asarray(inp["w_out"], np.float32)[0]),
        "rel_bias": np.ascontiguousarray(np.asarray(inp["rel_bias"], np.float32)),
        "w_gate": np.ascontiguousarray(np.asarray(inp["w_gate"], np.float32)[0]),
        "w_up": np.ascontiguousarray(np.asarray(inp["w_up"], np.float32)[0]),
        "convT": np.ascontiguousarray(np.asarray(inp["conv_w"], np.float32)[0].reshape(3, NFF, 128).transpose(2, 1, 0).reshape(128, NFF * 3)),
        "convb": _colT(np.asarray(inp["conv_b"])[0], NFF),
        "w_down": np.ascontiguousarray(np.asarray(inp["w_down"], np.float32)[0]),
        "ident": np.eye(128, dtype=np.float32),
        "tri": (np.arange(128)[:, None] <= np.arange(128)[None, :]).astype(np.float32),
        "wc": wc, "wcap": wcap, "hcap": hcap, "hv": hv, "ohb": ohb, "cz": cz,
    }
    return d


_NC_CACHE = {}


def kernel(**inputs):
    if "nc" not in _NC_CACHE:
        _NC_CACHE["nc"] = Builder().build()
    nc = _NC_CACHE["nc"]
    in_maps = []
    for core in range(8):
        b, j = core // 4, core % 4
        in_maps.append(make_core_inputs(inputs, b, j))
    res = run_bass_kernel_spmd(nc, in_maps, core_ids=list(range(8)))
    out = np.zeros((2, SEQ, D), np.float32)
    for core in range(8):
        b, j = core // 4, core % 4
        y = np.asarray(res.results[core]["y"], np.float32)
        for r in range(NTILE):
            t0 = TW * (4 * r + j)
            out[b, t0:t0 + TW] = y[TW * r:TW * r + TW]
    return out
```
